# Optimizing a Trainium2 kernel written in Bass

```python
import jax, jax.numpy as jnp
from jax import lax
import numpy as np

D_MODEL = 1024
BATCH = 16
SEQ = 2048
DEPTH = 1

GRID_W = 64
CTX_LEN = 256
D_MIX = D_MODEL
GLA_WIDTH = D_MIX // 2
POOL_WIDTH = D_MIX - GLA_WIDTH
GLA_HEADS = 4
GLA_DV = GLA_WIDTH // GLA_HEADS
GLA_DK = GLA_DV // 2
GLA_QK = GLA_HEADS * GLA_DK
GATE_RANK = 16
GATE_NORMALIZER = 16.0
GLA_CHUNK = 64
POOL_GROUPS = 4
POOL_GC = POOL_WIDTH // POOL_GROUPS
POOL_WINDOWS = (2, 4, 8, 16)
N_EXPERTS = 32
TOP_K = 4
D_FF = D_MODEL
SWIGLU_LIMIT = 7.0
SWIGLU_ALPHA = 1.702
EXPERT_BLOCK = 128
N_MOD = 6
EPS = 1e-6
D_IN = 2 * GLA_QK + 2 * GLA_WIDTH + 2 * GATE_RANK + POOL_WIDTH
SPLIT_POINTS = (GLA_QK, 2 * GLA_QK, 2 * GLA_QK + GLA_WIDTH, 2 * GLA_QK + 2 * GLA_WIDTH,
                2 * GLA_QK + 2 * GLA_WIDTH + GATE_RANK, 2 * GLA_QK + 2 * GLA_WIDTH + 2 * GATE_RANK)

kernel_name = "hybrid_gla_pool_moe_diffusion_layer"


def rmsnorm(x, w):
    xf = x.astype(jnp.float32)
    var = jnp.mean(xf * xf, axis=-1, keepdims=True)
    return (xf * lax.rsqrt(var + EPS) * w.astype(jnp.float32)).astype(x.dtype)


def modulate(h, shift, scale):
    return h * (1.0 + scale) + shift


def to_heads(t, d):
    b_, n, _ = t.shape
    return t.reshape(b_, n, GLA_HEADS, d).transpose(0, 2, 1, 3).astype(jnp.float32)


def mixer_inputs(p, w_gk_f, b_gk_f, w_gk_b, b_gk_b):
    q, k, v, g, r_f, r_b, pool_in = jnp.split(p, SPLIT_POINTS, axis=-1)
    gk_f = jax.nn.log_sigmoid((r_f @ w_gk_f + b_gk_f).astype(jnp.float32)) / GATE_NORMALIZER
    gk_b = jax.nn.log_sigmoid((r_b @ w_gk_b + b_gk_b).astype(jnp.float32)) / GATE_NORMALIZER
    return (to_heads(q, GLA_DK), to_heads(k, GLA_DK), to_heads(v, GLA_DV),
            to_heads(gk_f, GLA_DK), to_heads(gk_b, GLA_DK), g, pool_in)


def gla_chunked(q, k, v, gk, s0, with_output=True):
    b_, h_, t_, dk = q.shape
    dv = v.shape[-1]
    n_ch = t_ // GLA_CHUNK
    qc = q.reshape(b_, h_, n_ch, GLA_CHUNK, dk) * (dk ** -0.5)
    kc = k.reshape(b_, h_, n_ch, GLA_CHUNK, dk)
    vc = v.reshape(b_, h_, n_ch, GLA_CHUNK, dv)
    bcum = jnp.cumsum(gk.reshape(b_, h_, n_ch, GLA_CHUNK, dk), axis=3)
    b_last = bcum[:, :, :, -1:, :]
    u = jnp.einsum('bhnsk,bhnsv->bhnkv', kc * jnp.exp(b_last - bcum), vc)
    decay = jnp.exp(b_last[:, :, :, 0, :])

    def step(s, inp):
        d, un = inp
        return d[..., None] * s + un, s

    s_final, s_prev = lax.scan(step, s0, (jnp.moveaxis(decay, 2, 0), jnp.moveaxis(u, 2, 0)))
    if not with_output:
        return None, s_final
    s_prev = jnp.moveaxis(s_prev, 0, 2)
    b_mid = bcum[:, :, :, GLA_CHUNK // 2 - 1:GLA_CHUNK // 2, :]
    a = jnp.einsum('bhntk,bhnsk->bhnts', qc * jnp.exp(bcum - b_mid), kc * jnp.exp(b_mid - bcum))
    mask = jnp.tril(jnp.ones((GLA_CHUNK, GLA_CHUNK), dtype=bool))
    a = jnp.where(mask, a, 0.0)
    o = (jnp.einsum('bhnts,bhnsv->bhntv', a, vc)
         + jnp.einsum('bhntk,bhnkv->bhntv', qc * jnp.exp(bcum), s_prev))
    return o.reshape(b_, h_, t_, dv), s_final


def gla_bidir(q, k, v, gk_f, gk_b, s0_f, s0_b, with_output=True):
    flip = lambda t: jnp.flip(t, axis=2)
    o_f, s_f = gla_chunked(q, k, v, gk_f, s0_f, with_output)
    o_b, s_b = gla_chunked(flip(q), flip(k), flip(v), flip(gk_b), s0_b, with_output)
    o = o_f + flip(o_b) if with_output else None
    return o, s_f, s_b


def gla_output(o, g, w_norm):
    b_, _, n, _ = o.shape
    o = o.transpose(0, 2, 1, 3)
    o = o * lax.rsqrt(jnp.mean(o * o, axis=-1, keepdims=True) + EPS) * w_norm.astype(jnp.float32)
    gate = jax.nn.silu(g.astype(jnp.float32)).reshape(o.shape)
    return (o * gate).reshape(b_, n, GLA_WIDTH)


def window_bounds(pos, w, length):
    lo = w // 2
    hi = w - 1 - lo
    return jnp.clip(pos - lo, 0, length), jnp.clip(pos + hi + 1, 0, length)


def pool_grid(xp, rows):
    b_, n, _ = xp.shape
    xf = xp.astype(jnp.float32).reshape(b_, rows, GRID_W, POOL_GROUPS, POOL_GC)
    sat = jnp.pad(jnp.cumsum(jnp.cumsum(xf, axis=1), axis=2), ((0, 0), (1, 0), (1, 0), (0, 0), (0, 0)))
    r = jnp.arange(rows)
    col = jnp.arange(GRID_W)
    outs = []
    for gi, w in enumerate(POOL_WINDOWS):
        r0, r1 = window_bounds(r, w, rows)
        c0, c1 = window_bounds(col, w, GRID_W)
        s = sat[:, :, :, gi]
        s_r1, s_r0 = s[:, r1], s[:, r0]
        tot = s_r1[:, :, c1] - s_r0[:, :, c1] - s_r1[:, :, c0] + s_r0[:, :, c0]
        cnt = ((r1 - r0)[:, None] * (c1 - c0)[None, :]).astype(jnp.float32)
        outs.append(tot / cnt[None, :, :, None])
    mean = jnp.stack(outs, axis=3)
    return (mean - xf).reshape(b_, n, POOL_GROUPS, POOL_GC)


def pool_seq(xp):
    b_, n, _ = xp.shape
    xf = xp.astype(jnp.float32).reshape(b_, n, POOL_GROUPS, POOL_GC)
    cs = jnp.pad(jnp.cumsum(xf, axis=1), ((0, 0), (1, 0), (0, 0), (0, 0)))
    pos = jnp.arange(n)
    outs = []
    for gi, w in enumerate(POOL_WINDOWS):
        i0, i1 = window_bounds(pos, w, n)
        outs.append((cs[:, i1, gi] - cs[:, i0, gi]) / (i1 - i0).astype(jnp.float32)[None, :, None])
    return jnp.stack(outs, axis=2) - xf


def pool_project(pooled, w_pool, pool_scale):
    b_, n = pooled.shape[:2]
    y = jnp.einsum('bngc,gcd->bngd', pooled, w_pool.astype(jnp.float32)).reshape(b_, n, POOL_WIDTH)
    return y * pool_scale.astype(jnp.float32)


def moe(h, w_router, b_router, w_gu, b_gu, w_down, b_down):
    shp = h.shape
    xt = h.reshape(-1, D_MODEL)
    n_tok = xt.shape[0]
    n_assign = n_tok * TOP_K
    logits = (xt @ w_router + b_router).astype(jnp.float32)
    top_vals, top_idx = lax.top_k(logits, TOP_K)
    weights = jax.nn.softmax(top_vals, axis=-1)
    flat_e = top_idx.reshape(-1)
    flat_tok = jnp.repeat(jnp.arange(n_tok, dtype=jnp.int32), TOP_K)
    flat_w = weights.reshape(-1)
    order = jnp.argsort(flat_e)
    sorted_e = flat_e[order]
    counts = jnp.bincount(flat_e, length=N_EXPERTS)
    padded = (counts + EXPERT_BLOCK - 1) // EXPERT_BLOCK * EXPERT_BLOCK
    start_unpad = jnp.cumsum(counts) - counts
    pad_end = jnp.cumsum(padded)
    start_pad = pad_end - padded
    dest = start_pad[sorted_e] + (jnp.arange(n_assign) - start_unpad[sorted_e])
    n_pad = n_assign + N_EXPERTS * EXPERT_BLOCK
    n_blocks = n_pad // EXPERT_BLOCK
    buf_tok = jnp.full((n_pad,), n_tok, dtype=jnp.int32).at[dest].set(flat_tok[order])
    buf_w = jnp.zeros((n_pad,), jnp.float32).at[dest].set(flat_w[order])
    block_e = jnp.minimum(jnp.searchsorted(pad_end, jnp.arange(n_blocks) * EXPERT_BLOCK, side='right'),
                          N_EXPERTS - 1)
    x_pad = jnp.concatenate([xt, jnp.zeros((1, D_MODEL), xt.dtype)], axis=0)
    xb = x_pad[buf_tok].reshape(n_blocks, EXPERT_BLOCK, D_MODEL)

    def expert_block(args):
        xblk, e = args
        gu = xblk @ w_gu[e] + b_gu[e]
        gate, up = jnp.split(gu, 2, axis=-1)
        gate = jnp.minimum(gate, SWIGLU_LIMIT)
        up = jnp.clip(up, -SWIGLU_LIMIT, SWIGLU_LIMIT)
        act = (up + 1.0) * gate * jax.nn.sigmoid(SWIGLU_ALPHA * gate)
        return act @ w_down[e] + b_down[e]

    yb = lax.map(expert_block, (xb, block_e)).reshape(n_pad, D_MODEL)
    y = jnp.zeros((n_tok + 1, D_MODEL), yb.dtype).at[buf_tok].add(yb * buf_w[:, None].astype(yb.dtype))
    return y[:n_tok].reshape(shp)


def setup_inputs(seed: int = 0) -> dict:
    key = jax.random.key(seed)
    ks = jax.random.split(key, 32)
    nrm = lambda k, shp, s: jax.random.normal(k, shp, jnp.float32) * s
    L, D, E = DEPTH, D_MODEL, N_EXPERTS
    return {
        "x": nrm(ks[0], (BATCH, SEQ, D), 1.0),
        "c": nrm(ks[1], (BATCH, D), 1.0),
        "ctx": nrm(ks[2], (BATCH, CTX_LEN, D), 1.0),
        "c_ctx": nrm(ks[3], (D,), 1.0),
        "w_ada": nrm(ks[4], (L, D, N_MOD * D), 0.5 * D ** -0.5),
        "b_ada": nrm(ks[5], (L, N_MOD * D), 0.02),
        "norm_mix_w": 1.0 + nrm(ks[6], (L, D), 0.02),
        "norm_mlp_w": 1.0 + nrm(ks[7], (L, D), 0.02),
        "w_in": nrm(ks[8], (L, D, D_IN), D ** -0.5),
        "w_gk_f": nrm(ks[9], (L, GATE_RANK, GLA_QK), GATE_RANK ** -0.5),
        "b_gk_f": jax.random.uniform(ks[10], (L, GLA_QK), jnp.float32, -2.0, 4.0),
        "w_gk_b": nrm(ks[11], (L, GATE_RANK, GLA_QK), GATE_RANK ** -0.5),
        "b_gk_b": jax.random.uniform(ks[12], (L, GLA_QK), jnp.float32, -2.0, 4.0),
        "gla_norm_w": 1.0 + nrm(ks[13], (L, GLA_DV), 0.02),
        "w_pool": nrm(ks[14], (L, POOL_GROUPS, POOL_GC, POOL_GC), POOL_GC ** -0.5),
        "pool_scale": 1.0 + nrm(ks[15], (L, POOL_WIDTH), 0.1),
        "w_out": nrm(ks[16], (L, D_MIX, D), D_MIX ** -0.5),
        "w_router": nrm(ks[17], (L, D, E), D ** -0.5),
        "b_router": nrm(ks[18], (L, E), 0.01),
        "w_gu": nrm(ks[19], (L, E, D, 2 * D_FF), D ** -0.5),
        "b_gu": nrm(ks[20], (L, E, 2 * D_FF), 0.02),
        "w_down": nrm(ks[21], (L, E, D_FF, D), D_FF ** -0.5),
        "b_down": nrm(ks[22], (L, E, D), 0.02),
        "final_norm_w": 1.0 + nrm(ks[23], (D,), 0.02),
    }


def reference(x, c, ctx, c_ctx, w_ada, b_ada, norm_mix_w, norm_mlp_w, w_in, w_gk_f, b_gk_f, w_gk_b,
              b_gk_b, gla_norm_w, w_pool, pool_scale, w_out, w_router, b_router, w_gu, b_gu, w_down,
              b_down, final_norm_w):
    h, hc = x, ctx
    b_, n_lat, _ = x.shape
    rows = n_lat // GRID_W
    for l in range(DEPTH):
        last = l == DEPTH - 1
        mod_x = (jax.nn.silu(c) @ w_ada[l] + b_ada[l])[:, None, :]
        mod_c = jax.nn.silu(c_ctx) @ w_ada[l] + b_ada[l]
        sh1, sc1, g1, sh2, sc2, g2 = jnp.split(mod_x, N_MOD, axis=-1)
        csh1, csc1, cg1, csh2, csc2, cg2 = jnp.split(mod_c, N_MOD, axis=-1)

        p_x = modulate(rmsnorm(h, norm_mix_w[l]), sh1, sc1) @ w_in[l]
        p_c = modulate(rmsnorm(hc, norm_mix_w[l]), csh1, csc1) @ w_in[l]
        qx, kx, vx, gkfx, gkbx, gx, poolx = mixer_inputs(p_x, w_gk_f[l], b_gk_f[l], w_gk_b[l], b_gk_b[l])
        qc, kc, vc, gkfc, gkbc, gc, poolc = mixer_inputs(p_c, w_gk_f[l], b_gk_f[l], w_gk_b[l], b_gk_b[l])

        s0 = jnp.zeros((b_, GLA_HEADS, GLA_DK, GLA_DV), jnp.float32)
        o_c, s_f, s_b = gla_bidir(qc, kc, vc, gkfc, gkbc, s0, s0, with_output=not last)
        o_x, _, _ = gla_bidir(qx, kx, vx, gkfx, gkbx, s_f, s_b)

        mix_x = jnp.concatenate([gla_output(o_x, gx, gla_norm_w[l]),
                                 pool_project(pool_grid(poolx, rows), w_pool[l], pool_scale[l])], axis=-1)
        h = h + g1 * (mix_x.astype(h.dtype) @ w_out[l])

        if not last:
            mix_c = jnp.concatenate([gla_output(o_c, gc, gla_norm_w[l]),
                                     pool_project(pool_seq(poolc), w_pool[l], pool_scale[l])], axis=-1)
            hc = hc + cg1 * (mix_c.astype(hc.dtype) @ w_out[l])
            hc = hc + cg2 * moe(modulate(rmsnorm(hc, norm_mlp_w[l]), csh2, csc2),
                                w_router[l], b_router[l], w_gu[l], b_gu[l], w_down[l], b_down[l])

        h = h + g2 * moe(modulate(rmsnorm(h, norm_mlp_w[l]), sh2, sc2),
                         w_router[l], b_router[l], w_gu[l], b_gu[l], w_down[l], b_down[l])
    return rmsnorm(h, final_norm_w)
```

```python
import numpy as np
import concourse.bass as bass
import concourse.mybir as mybir
from concourse.bass_utils import run_bass_kernel_spmd

F32 = mybir.dt.float32
BF16 = mybir.dt.bfloat16
I32 = mybir.dt.int32
ALU = mybir.AluOpType
AF = mybir.ActivationFunctionType
ET = mybir.EngineType

NCORES = 8
D = 1024
NT = 4096
NTILE = 32
NCTX = 512
E = 32
NBMAX = 32
CAP = NBMAX * 128
EPS = 1e-6
POOL_W = (2, 4, 8, 16)


class Buf:
    __slots__ = ("w", "r")

    def __init__(self):
        self.w = None
        self.r = {}


class T:
    def __init__(self, t):
        self.t = t
        self.b = Buf()

    def __getitem__(self, k):
        return self.t[k]


class FW:
    ENG = ("pe", "act", "dve", "pool", "sp")

    def __init__(self, nc, n_dma_sems=(14, 10)):
        self.nc = nc
        self.eng = {"pe": nc.tensor, "act": nc.scalar, "dve": nc.vector, "pool": nc.gpsimd, "sp": nc.sync}
        self.sem = {k: nc.alloc_semaphore(f"s_{k}") for k in self.ENG}
        self.cnt = {k: 0 for k in self.ENG}
        tot = sum(n_dma_sems)
        self.dsem = [nc.alloc_semaphore(f"d_{i}") for i in range(tot)]
        self.dcnt = [0] * tot
        self.dpool = {}
        self.downer = {}
        o = 0
        for q, n in zip(("sp", "pool"), n_dma_sems):
            self.dpool[q] = [list(range(o, o + n)), 0]
            for i in range(o, o + n):
                self.downer[i] = q
            o += n
        self.seen = {k: {} for k in self.ENG}
        self.if_stack = []
        self.n_ins = 0

    def _semh(self, key):
        return self.sem[key] if isinstance(key, str) else self.dsem[key]

    def _deps(self, reads, writes):
        deps = {}

        def add(k, v):
            if deps.get(k, 0) < v:
                deps[k] = v
        for t in reads:
            if t.b.w is not None:
                add(*t.b.w)
        for t in writes:
            if t.b.w is not None:
                add(*t.b.w)
            for k, v in t.b.r.items():
                add(k, v)
        return deps

    def _need(self, e, deps, skip_self=False):
        need = []
        for k, v in deps.items():
            if skip_self and k == e:
                continue
            if self.seen[e].get(k, 0) < v:
                need.append((k, v))
                self.seen[e][k] = v
        return need

    def op(self, e, fn, r=(), w=()):
        need = self._need(e, self._deps(r, w), skip_self=(e == "pe"))
        eng = self.eng[e]
        for k, v in need[1:]:
            eng.wait_ge(self._semh(k), v)
        ins = fn(eng)
        if need:
            ins._wait_ge(self._semh(need[0][0]), need[0][1])
        self.cnt[e] += 1
        ins.then_inc(self.sem[e], 1)
        c = self.cnt[e]
        for t in w:
            t.b.w = (e, c)
            t.b.r = {}
        for t in r:
            if t.b.r.get(e, 0) < c:
                t.b.r[e] = c
        self.n_ins += 1
        return ins

    def dma(self, q, fn, r=(), w=()):
        deps = self._deps(r, w)
        pl = self.dpool[q]
        si = pl[0][pl[1]]
        pl[1] = (pl[1] + 1) % len(pl[0])
        if self.dcnt[si] > 0:
            deps[si] = max(deps.get(si, 0), self.dcnt[si])
        need = self._need(q, deps)
        eng = self.eng[q]
        for k, v in need[1:]:
            eng.wait_ge(self._semh(k), v)
        ins = fn(eng)
        if need:
            ins._wait_ge(self._semh(need[0][0]), need[0][1])
        self.dcnt[si] += 16
        ins.then_inc(self.dsem[si], 16)
        v = self.dcnt[si]
        for t in w:
            t.b.w = (si, v)
            t.b.r = {}
        for t in r:
            t.b.r[si] = v
        self.n_ins += 1
        return ins

    def sync_to(self, e, reads):
        for k, v in self._need(e, self._deps(reads, ())):
            self.eng[e].wait_ge(self._semh(k), v)

    def wait_all(self, e):
        eng = self.eng[e]
        for k in self.ENG:
            if k != e and self.cnt[k] > self.seen[e].get(k, 0):
                eng.wait_ge(self.sem[k], self.cnt[k])
                self.seen[e][k] = self.cnt[k]
        for i, v in enumerate(self.dcnt):
            if v > self.seen[e].get(i, 0):
                eng.wait_ge(self.dsem[i], v)
                self.seen[e][i] = v

    def barrier(self):
        for e in self.ENG:
            self.wait_all(e)

    def begin_if_cmp(self, regs, val, op):
        st = dict(cnt=dict(self.cnt), dcnt=list(self.dcnt), seen={k: dict(v) for k, v in self.seen.items()})
        g = self.nc.If_cmp(regs, val, op)
        g.__enter__()
        st["g"] = g
        self.if_stack.append(st)

    def end_if(self):
        st = self.if_stack.pop()
        st["g"].__exit__(None, None, None)
        g = self.nc.Else()
        g.__enter__()
        for e in self.ENG:
            eng = self.eng[e]
            d = self.cnt[e] - st["cnt"][e]
            if d > 0:
                eng.wait_ge(self.sem[e], st["cnt"][e])
                eng.sem_inc(self.sem[e], d)
            for si in range(len(self.dsem)):
                dd = self.dcnt[si] - st["dcnt"][si]
                if dd > 0 and self.downer[si] == e:
                    eng.wait_ge(self.dsem[si], st["dcnt"][si])
                    eng.sem_inc(self.dsem[si], dd)
        g.__exit__(None, None, None)
        self.seen = st["seen"]


def _window(pos, w, length):
    lo = w // 2
    hi = w - 1 - lo
    return np.clip(pos - lo, 0, length), np.clip(pos + hi + 1, 0, length)


def _pool_consts():
    mats, index, keymap = [], {}, {}
    invcnt = np.zeros((128, 64), np.float32)
    p = np.arange(128)
    for g, w in enumerate(POOL_W):
        lo = w // 2
        hi = w - 1 - lo
        for i in range(16):
            rt = 2 * i + p // 64
            ct = p % 64
            r0, r1 = _window(rt, w, 32)
            c0, c1 = _window(ct, w, 64)
            cnt = ((r1 - r0) * (c1 - c0)).astype(np.float32)
            invcnt[:, i * 4 + g] = 1.0 / cnt
            for j in range(16):
                rs = 2 * j + p // 64
                cs = p % 64
                m = ((rs[:, None] >= rt[None, :] - lo) & (rs[:, None] <= rt[None, :] + hi) &
                     (cs[:, None] >= ct[None, :] - lo) & (cs[:, None] <= ct[None, :] + hi)).astype(np.float32)
                if not m.any():
                    continue
                if i == j:
                    m = m - np.diag(cnt)
                key = m.tobytes()
                if key not in keymap:
                    keymap[key] = len(mats)
                    mats.append(m)
                index[(g, i, j)] = keymap[key]
    return mats, index, invcnt


_POOL_MATS, _POOL_INDEX, _INVCNT = _pool_consts()
NPM = len(_POOL_MATS)

CF_ID, CF_M1F, CF_M1B, CF_INV, CF_NEG, CF_ONES, CF_SENT, CF_ECAP, CF_TOK = 0, 128, 256, 384, 448, 449, 577, 578, 610
CF_N = 611
CB_ID, CB_MF, CB_MB, CB_SU, CB_ONES, CB_POOL = 0, 128, 256, 384, 512, 640
CB_N = CB_POOL + 128 * NPM


def _consts():
    import ml_dtypes
    s = np.arange(128)[:, None]
    t = np.arange(128)[None, :]
    cf = np.zeros((128, CF_N), np.float32)
    cf[:, CF_ID:CF_ID + 128] = np.eye(128)
    cf[:, CF_M1F:CF_M1F + 128] = (s > t) / 16.0
    cf[:, CF_M1B:CF_M1B + 128] = (s < t) / 16.0
    cf[:, CF_INV:CF_INV + 64] = _INVCNT
    cf[:, CF_NEG] = -1.0 / 16.0
    cf[:, CF_ONES:CF_ONES + 128] = 1.0
    cf[:, CF_SENT] = NT + np.arange(128)
    cf[:, CF_ECAP:CF_ECAP + 32] = (np.arange(32) * CAP)[None, :]
    cf[:, CF_TOK] = np.arange(128)
    cb = np.zeros((128, CB_N), np.float32)
    cb[:, CB_ID:CB_ID + 128] = np.eye(128)
    cb[:, CB_MF:CB_MF + 128] = (s <= t)
    cb[:, CB_MB:CB_MB + 128] = (s >= t)
    cb[:, CB_SU:CB_SU + 128] = (s < t)
    cb[:, CB_ONES:CB_ONES + 128] = 1.0
    for m, mat in enumerate(_POOL_MATS):
        cb[:, CB_POOL + 128 * m:CB_POOL + 128 * (m + 1)] = mat
    return cf, cb.astype(ml_dtypes.bfloat16)


def _list_init():
    r = np.arange(128 * E * NBMAX)
    img = np.stack([NT + r % 128, np.zeros_like(r)], axis=1).astype(np.int32)
    return np.ascontiguousarray(img.reshape(128, E * NBMAX * 2))


class _Stop(Exception):
    pass


def build_program(nbmax=NBMAX, phases=4, stop=None, debug=False):
    from contextlib import ExitStack

    def CHK(name):
        if stop == name:
            raise _Stop()

    nc = bass.Bass("TRN2", target_bir_lowering=False)
    fw = FW(nc)
    OP = fw.op

    def din(name, shape, dt=F32):
        return nc.dram_tensor(name, list(shape), dt, kind="ExternalInput").ap()

    x_d = din("x", [NT, D]); ctx_d = din("ctx", [NCTX, D]); cT_d = din("cT", [128, 8, 3])
    wada_d = din("w_ada", [D, 6 * D]); bada_d = din("b_ada", [1, 6 * D])
    nmw_d = din("nmwT", [128, 8]); nmlp_d = din("nmlp_bc", [128, D]); fnw_d = din("fnw_bc", [128, D])
    win_d = din("w_in", [D, 2080]); wgk_d = din("wgk", [32, 512]); bgk_d = din("bgk", [1, 512])
    gnw_d = din("gnw_bc", [128, 512]); wpool_d = din("w_pool", [4, 128, 128]); psc_d = din("psc_bc", [128, 512])
    wout_d = din("w_out", [D, D]); wr_d = din("w_router", [D, E]); br_d = din("br_bc", [128, E])
    wgu_d = din("w_gu", [E, D, 2 * D]); bgu_d = din("b_gu", [E, 2 * D]); wdn_d = din("w_down", [E, D, D]); bdn_d = din("b_down", [E, D])
    cf_d = din("cf", [128, CF_N]); cb_d = din("cb", [128, CB_N], BF16)
    linit_d = din("linit", [128, E * NBMAX * 2], I32)
    out_d = nc.dram_tensor("out", [NT, D], F32, kind="ExternalOutput").ap()
    SK = "ExternalOutput" if debug else "Internal"
    h1_d = nc.dram_tensor("h1s", [NT, D], F32, kind=SK).ap()
    x2_d = nc.dram_tensor("x2s", [NT + 128, D], BF16, kind=SK).ap()
    yacc_d = nc.dram_tensor("yacc", [NT + 128, D], F32, kind=SK).ap()
    lists_d = nc.dram_tensor("lists", [128 * E * NBMAX, 2], I32, kind=SK).ap()
    nblk_d = nc.dram_tensor("nblk", [1, E], I32, kind=SK).ap()
    mod_d = nc.dram_tensor("mods", [3, 6 * D], F32, kind=SK).ap()
    H1D, X2D, YACC, LISTS, NBLKD, MODD = (T(None) for _ in range(6))
    wgu16_d = nc.dram_tensor("wgu16", [E, D, 2 * D], BF16).ap()
    wdn16_d = nc.dram_tensor("wdn16", [E, D, D], BF16).ap()
    WCV = [[T(None), T(None)] for _ in range(E)]
    conv_todo = [(e_, h_) for e_ in range(E) for h_ in range(2)]

    def conv_step(pace=None):
        if not conv_todo:
            return
        e_, h_ = conv_todo.pop(0)
        if pace is not None:
            fw.sync_to("pool", pace)
        if h_ == 0:
            fw.dma("pool", lambda q: q.dma_start(out=wgu16_d[e_], in_=wgu_d[e_]), w=[WCV[e_][0]])
        else:
            fw.dma("pool", lambda q: q.dma_start(out=wdn16_d[e_], in_=wdn_d[e_]), w=[WCV[e_][1]])
    dbg_d = nc.dram_tensor("dbg", [128, 8192], F32, kind=SK).ap()
    DBG = T(None)

    def DUMP(t, ap, off, n):
        fw.dma("sp", lambda q: q.dma_start(out=dbg_d[0:ap.shape[0], off:off + n], in_=ap), r=[t], w=[DBG])

    _uid = [0]

    def sb(st, name, shape, dt):
        _uid[0] += 1
        return T(st.enter_context(nc.sbuf_tensor(f"sb_{name}_{_uid[0]}", list(shape), dt)))

    root = ExitStack()
    psum = nc.alloc_psum_tensor("psum", [128, 4096], F32)
    P = [T(None) for _ in range(8)]

    def pb(i, lo=0, hi=512):
        return psum[:, i * 512 + lo:i * 512 + hi]

    def pbb(i):
        return psum[:, i * 512:(i + 1) * 512].bitcast(BF16)

    cf = sb(root, "cf", [128, CF_N], F32); cb = sb(root, "cb", [128, CB_N], BF16)
    fw.dma("sp", lambda q: q.dma_start(out=cf[:], in_=cf_d), w=[cf])
    fw.dma("sp", lambda q: q.dma_start(out=cb[:], in_=cb_d), w=[cb])
    ident = cf[:, CF_ID:CF_ID + 128]
    identb = cb[:, CB_ID:CB_ID + 128]
    a1fm = sb(root, "a1fm", [128, 3, 8], F32); b1fm = sb(root, "b1fm", [128, 3, 8], F32)
    carry = sb(root, "carry", [128, E], F32)
    OP("pool", lambda e: e.memset(carry[:], 0.0), w=[carry])

    def rstd_from_ss(ss, rs, n):
        OP("dve", lambda e: e.tensor_scalar(out=rs, in0=ss, scalar1=1.0 / n, scalar2=EPS, op0=ALU.mult, op1=ALU.add), r=[tmpv], w=[tmpv])
        OP("act", lambda e: e.activation(out=rs, in_=rs, func=AF.Sqrt), r=[tmpv], w=[tmpv])
        OP("dve", lambda e: e.reciprocal(out=rs, in_=rs), r=[tmpv], w=[tmpv])

    tmpv = sb(root, "tmpv", [128, 64], F32)
    z4 = sb(root, "z4", [128, 4], F32)
    nbi = sb(root, "nbi", [128, E], I32)
    OP("pool", lambda e: e.memset(z4[:], 0.0), w=[z4])

    try:
        with ExitStack() as st:
            cTs = sb(st, "cTs", [128, 8, 3], F32)
            wa = [sb(st, f"wa{i}", [128, 8, 512], F32) for i in range(2)]
            badab = sb(st, "badab", [3, 6 * D], F32)
            modrow = sb(st, "modrow", [3, 6 * D], F32)
            sfm = sb(st, "sfm", [128, 3, 8], F32)
            nmw = sb(st, "nmw", [128, 8], F32)
            fw.dma("sp", lambda q: q.dma_start(out=cTs[:], in_=cT_d), w=[cTs])
            fw.dma("sp", lambda q: q.dma_start(out=badab[:], in_=bada_d.partition_broadcast(3)), w=[badab])
            fw.dma("sp", lambda q: q.dma_start(out=nmw[:], in_=nmw_d), w=[nmw])
            OP("act", lambda e: e.activation(out=cTs[:], in_=cTs[:], func=AF.Silu), r=[cTs], w=[cTs])
            wav = wada_d.rearrange("(k p) n -> p k n", p=128)
            for n in range(12):
                wt = wa[n % 2]
                fw.dma("sp", lambda q: q.dma_start(out=wt[:], in_=wav[:, :, n * 512:(n + 1) * 512]), w=[wt])
                for k in range(8):
                    OP("pe", lambda e: e.matmul(pb(0)[0:3, :], lhsT=cTs[:, k, :], rhs=wt[:, k, :], start=(k == 0), stop=(k == 7)),
                       r=[cTs, wt], w=[P[0]])
                OP("dve", lambda e: e.tensor_tensor(out=modrow[0:3, n * 512:(n + 1) * 512], in0=pb(0)[0:3, :],
                                                    in1=badab[0:3, n * 512:(n + 1) * 512], op=ALU.add), r=[P[0], badab], w=[modrow])
            fw.dma("sp", lambda q: q.dma_start(out=mod_d, in_=modrow[0:3, :]), r=[modrow], w=[MODD])
            for j in range(3):
                fw.dma("sp", lambda q: q.dma_start(out=b1fm[:, j, :], in_=mod_d[j, 0:D].rearrange("(c p) -> p c", p=128),
                                                   allow_slow_non_contiguous=True), r=[MODD], w=[b1fm])
                fw.dma("sp", lambda q: q.dma_start(out=sfm[:, j, :], in_=mod_d[j, D:2 * D].rearrange("(c p) -> p c", p=128),
                                                   allow_slow_non_contiguous=True), r=[MODD], w=[sfm])
                OP("dve", lambda e: e.scalar_tensor_tensor(out=a1fm[:, j, :], in0=sfm[:, j, :], scalar=1.0, in1=nmw[:],
                                                           op0=ALU.add, op1=ALU.mult), r=[sfm, nmw], w=[a1fm])
            fw.barrier()
        CHK("p0")

        with ExitStack() as st:
            wpoolb = sb(st, "wpoolb", [128, 4, 128], BF16)
            wrb = sb(st, "wrb", [128, 8, E], BF16)
            wgk = sb(st, "wgk", [32, 512], F32); bgk = sb(st, "bgk", [1, 512], F32)
            gnwb = sb(st, "gnwb", [128, 512], F32); pscb = sb(st, "pscb", [128, 512], F32)
            brb = sb(st, "brb", [128, E], F32)
            fw.dma("pool", lambda q: q.dma_start(out=wpoolb[:], in_=wpool_d.rearrange("g c d -> c g d")), w=[wpoolb])
            fw.dma("pool", lambda q: q.dma_start(out=wrb[:], in_=wr_d.rearrange("(k p) n -> p k n", p=128)), w=[wrb])
            for tt, dd in ((wgk, wgk_d), (bgk, bgk_d), (gnwb, gnw_d), (pscb, psc_d), (brb, br_d)):
                fw.dma("sp", lambda q: q.dma_start(out=tt[:], in_=dd), w=[tt])
            fw.dma("sp", lambda q: q.dma_start(out=lists_d.rearrange("(q r) two -> q (r two)", q=128), in_=linit_d), w=[LISTS])
            zero_todo = []
            CHK("init")

            vS = [sb(st, f"vS{i}", [128, 512], BF16) for i in range(18)]
            kdS = [sb(st, f"kdS{i}", [128, 512], BF16) for i in range(18)]
            decS = [sb(st, f"decS{i}", [128, 4], F32) for i in range(18)]
            gwS = [sb(st, f"gwS{i}", [128, 512], BF16) for i in range(16)]
            plS = [sb(st, f"plS{i}", [128, 512], BF16) for i in range(16)]
            qTS = [sb(st, f"qTS{i}", [128, 4, 128], BF16) for i in range(16)]
            kTS = [sb(st, f"kTS{i}", [128, 4, 128], BF16) for i in range(16)]
            dSS = [sb(st, f"dSS{i}", [128, 4, 128], BF16) for i in range(16)]
            S = sb(st, "S", [128, 4, 128], F32)
            ones1 = cf[0:1, CF_ONES:CF_ONES + 128]
            SLOT_BANKS = ((0, 2, 4, 6), (1, 3, 5, 7))

            def rstd_ss(tv, ss, rs, n):
                OP("dve", lambda e: e.tensor_scalar(out=rs, in0=ss, scalar1=1.0 / n, scalar2=EPS, op0=ALU.mult, op1=ALU.add), r=[tv], w=[tv])
                OP("act", lambda e: e.activation(out=rs, in_=rs, func=AF.Sqrt), r=[tv], w=[tv])
                OP("dve", lambda e: e.reciprocal(out=rs, in_=rs), r=[tv], w=[tv])

            def run_pipelined(make_gen, items, nslots=2, gap=6):
                items = list(items)
                active = []
                since = gap
                while items or active:
                    if items and len(active) < nslots and since >= gap:
                        used = {s_ for s_, _ in active}
                        slot = [s_ for s_ in range(nslots) if s_ not in used][0]
                        active.append((slot, make_gen(items.pop(0), slot)))
                        since = 0
                    for ent in list(active):
                        try:
                            next(ent[1])
                        except StopIteration:
                            active.remove(ent)
                    since += 1

            def u_mm(si, bank):
                for h in range(4):
                    OP("pe", lambda e: e.matmul(pb(bank, h * 128, (h + 1) * 128), lhsT=kdS[si][:, h * 128:(h + 1) * 128],
                                                rhs=vS[si][:, h * 128:(h + 1) * 128], start=True, stop=True), r=[kdS[si], vS[si]], w=[P[bank]])

            def state_update(si, bank, lo, hi, store=None):
                for h in range(4):
                    if store is not None:
                        OP("act", lambda e: e.activation(out=store[lo:hi, h, :], in_=S[lo:hi, h, :], func=AF.Copy, scale=decS[si][lo:hi, h:h + 1]),
                           r=[S, decS[si]], w=[store])
                    OP("dve", lambda e: e.scalar_tensor_tensor(out=S[lo:hi, h, :], in0=S[lo:hi, h, :], scalar=decS[si][lo:hi, h:h + 1],
                                                               in1=pb(bank, h * 128, (h + 1) * 128)[lo:hi, :], op0=ALU.mult, op1=ALU.add),
                       r=[S, decS[si], P[bank]], w=[S])

            for b in range(2):
              with ExitStack() as sp_:
                winb = sb(sp_, "winb", [128, 8, 2080], BF16)
                fw.dma("pool", lambda q: q.dma_start(out=winb[:], in_=win_d.rearrange("(k p) n -> p k n", p=128)), w=[winb])
                W = []
                for s_ in range(2):
                    W.append(dict(
                        xmT=sb(sp_, f"xmT{s_}", [128, 8, 128], BF16),
                        qk=sb(sp_, f"qk{s_}", [128, 512], F32), rsb=sb(sp_, f"rsb{s_}", [128, 32], F32), rT=sb(sp_, f"rT{s_}", [32, 128], F32),
                        Lt=sb(sp_, f"Lt{s_}", [128, 512], F32), Em=sb(sp_, f"Em{s_}", [128, 512], F32), qt=sb(sp_, f"qt{s_}", [128, 512], BF16),
                        junk=sb(sp_, f"junk{s_}", [128, D], BF16), tv=sb(sp_, f"tvp{s_}", [128, 8], F32)))

                xring = [sb(sp_, f"xring{i_}", [128, D], F32) for i_ in range(3)]
                xsrc = {}
                xissued = set()

                def x_issue(g):
                    if g in xissued or g not in xsrc:
                        return
                    xissued.add(g)
                    fw.dma("sp", lambda q: q.dma_start(out=xring[g % 3][:], in_=xsrc[g]), w=[xring[g % 3]])

                def prep_gen(item, slot):
                    g, src, is_ctx, si, a1, b1 = item
                    w_ = W[slot]
                    b0, b1k, b2, b3 = SLOT_BANKS[slot]
                    xmT, qk, rsb, rT, Lt, Em, qt, junk, tv = (w_[k] for k in ("xmT", "qk", "rsb", "rT", "Lt", "Em", "qt", "junk", "tv"))
                    Ep, gsil = Lt, Em
                    xi = xring[g % 3]
                    x_issue(g)
                    x_issue(g + 1)
                    OP("act", lambda e: e.activation(out=junk[:], in_=xi[:], func=AF.Square, accum_out=tv[:, 0:1]), r=[xi], w=[junk, tv])
                    rstd_ss(tv, tv[:, 0:1], tv[:, 1:2], D)
                    OP("act", lambda e: e.activation(out=xi[:], in_=xi[:], func=AF.Copy, scale=tv[:, 1:2]), r=[xi, tv], w=[xi])
                    yield
                    conv_step(pace=[xi])
                    for hf in range(2):
                        for c in range(4):
                            k = hf * 4 + c
                            OP("pe", lambda e: e.transpose(out=pb(b0, c * 128, (c + 1) * 128), in_=xi[:, k * 128:(k + 1) * 128], identity=ident),
                               r=[xi, cf], w=[P[b0]])
                        for c in range(4):
                            k = hf * 4 + c
                            OP("act", lambda e: e.activation(out=xmT[:, k, :], in_=pb(b0, c * 128, (c + 1) * 128), func=AF.Identity,
                                                             scale=a1[:, k:k + 1], bias=b1[:, k:k + 1]), r=[P[b0], a1fm, b1fm], w=[xmT])
                        yield
                    groups = [(0, 512, "qk"), (512, 1024, "v")] + ([] if is_ctx else [(1024, 1536, "g"), (1536, 2048, "pool")]) + [(2048, 2080, "r")]
                    for gi, (lo, hi, nm) in enumerate(groups):
                        bk = b1k
                        for k in range(8):
                            OP("pe", lambda e: e.matmul(pb(bk, 0, hi - lo), lhsT=xmT[:, k, :], rhs=winb[:, k, lo:hi], start=(k == 0), stop=(k == 7)),
                               r=[xmT, winb], w=[P[bk]])
                        if nm == "qk":
                            OP("dve", lambda e: e.tensor_copy(out=qk[:], in_=pb(bk)), r=[P[bk]], w=[qk])
                        elif nm == "v":
                            OP("act", lambda e: e.activation(out=vS[si][:], in_=pb(bk), func=AF.Copy), r=[P[bk]], w=[vS[si]])
                        elif nm == "g":
                            OP("act", lambda e: e.activation(out=gsil[:], in_=pb(bk), func=AF.Silu), r=[P[bk]], w=[gsil])
                            OP("dve", lambda e: e.tensor_tensor(out=gwS[si][:], in0=gsil[:], in1=gnwb[:], op=ALU.mult), r=[gsil, gnwb], w=[gwS[si]])
                        elif nm == "pool":
                            OP("dve", lambda e: e.tensor_copy(out=plS[si][:], in_=pb(bk)), r=[P[bk]], w=[plS[si]])
                        else:
                            OP("dve", lambda e: e.tensor_copy(out=rsb[:], in_=pb(bk, 0, 32)), r=[P[bk]], w=[rsb])
                        yield
                    OP("pe", lambda e: e.transpose(out=pb(b2, 0, 128)[0:32, :], in_=rsb[:, 0:32], identity=ident), r=[rsb, cf], w=[P[b2]])
                    OP("dve", lambda e: e.tensor_copy(out=rT[:], in_=pb(b2, 0, 128)[0:32, :]), r=[P[b2]], w=[rT])
                    OP("pe", lambda e: e.matmul(pb(b2), lhsT=rT[:], rhs=wgk[:], start=True, stop=False), r=[rT, wgk], w=[P[b2]])
                    OP("pe", lambda e: e.matmul(pb(b2), lhsT=ones1, rhs=bgk[:], start=False, stop=True), r=[cf, bgk], w=[P[b2]])
                    OP("act", lambda e: e.activation(out=Lt[:], in_=pb(b2), func=AF.Exp, scale=-1.0), r=[P[b2]], w=[Lt])
                    OP("act", lambda e: e.activation(out=Lt[:], in_=Lt[:], func=AF.Ln, bias=1.0), r=[Lt], w=[Lt])
                    yield
                    for h in range(4):
                        for d in range(2):
                            m1 = cf[:, (CF_M1F if d == 0 else CF_M1B):(CF_M1F if d == 0 else CF_M1B) + 128]
                            c0 = h * 128 + d * 64
                            OP("pe", lambda e: e.matmul(pb(b2, c0, c0 + 64), lhsT=m1, rhs=Lt[:, c0:c0 + 64], start=True, stop=True),
                               r=[cf, Lt], w=[P[b2]])
                    for h in range(4):
                        OP("pe", lambda e: e.matmul(pb(b3, h, h + 1), lhsT=Lt[:, h * 128:(h + 1) * 128], rhs=cf[:, CF_NEG:CF_NEG + 1],
                                                    start=True, stop=True), r=[Lt, cf], w=[P[b3]])
                    OP("act", lambda e: e.activation(out=decS[si][:], in_=pb(b3, 0, 4), func=AF.Exp), r=[P[b3]], w=[decS[si]])
                    OP("act", lambda e: e.activation(out=Em[:], in_=pb(b2), func=AF.Exp, scale=-1.0), r=[P[b2]], w=[Em])
                    k4 = qk[:, 256:512].rearrange("p (h k) -> p h k", h=4)
                    for d in range(2):
                        OP("dve", lambda e: e.tensor_tensor(
                            out=kdS[si][:].rearrange("p (h d k) -> p h d k", h=4, d=2)[:, :, d, :], in0=k4,
                            in1=Em[:].rearrange("p (h d k) -> p h d k", h=4, d=2)[:, :, d, :], op=ALU.mult), r=[qk, Em], w=[kdS[si]])
                    yield
                    if is_ctx:
                        u_mm(si, b3)
                        return
                    OP("act", lambda e: e.activation(out=Ep[:], in_=pb(b2), func=AF.Exp), r=[P[b2]], w=[Ep])
                    q4 = qk[:, 0:256].rearrange("p (h k) -> p h k", h=4)
                    for d in range(2):
                        OP("dve", lambda e: e.scalar_tensor_tensor(
                            out=qt[:].rearrange("p (h d k) -> p h d k", h=4, d=2)[:, :, d, :], in0=q4, scalar=0.125,
                            in1=Ep[:].rearrange("p (h d k) -> p h d k", h=4, d=2)[:, :, d, :], op0=ALU.mult, op1=ALU.mult), r=[qk, Ep], w=[qt])
                    yield
                    for srcT, dstT, bk, eng in ((qt, qTS[si], b0, "act"), (kdS[si], kTS[si], b1k, "dve")):
                        for h in range(4):
                            OP("pe", lambda e: e.transpose(out=pbb(bk)[:, h * 128:(h + 1) * 128], in_=srcT[:, h * 128:(h + 1) * 128], identity=identb),
                               r=[srcT, cb], w=[P[bk]])
                        if eng == "act":
                            OP("act", lambda e: e.activation(out=dstT[:].rearrange("p h t -> p (h t)"), in_=pbb(bk)[:, 0:512], func=AF.Copy), r=[P[bk]], w=[dstT])
                        else:
                            OP("dve", lambda e: e.tensor_copy(out=dstT[:].rearrange("p h t -> p (h t)"), in_=pbb(bk)[:, 0:512]), r=[P[bk]], w=[dstT])
                    yield
                    u_mm(si, b3)
                    state_update(si, b3, 0, 64, store=dSS[si])

                OP("pool", lambda e: e.memset(S[:], 0.0), w=[S])
                for j in range(2):
                    xsrc[j] = ctx_d[b * 256 + j * 128: b * 256 + (j + 1) * 128, :]
                for i in range(16):
                    xsrc[2 + i] = x_d[b * 2048 + i * 128: b * 2048 + (i + 1) * 128, :]
                run_pipelined(prep_gen, [(j, xsrc[j], True, 16 + j, a1fm[:, 2, :], b1fm[:, 2, :]) for j in range(2)], gap=0)
                for j in (0, 1):
                    state_update(16 + j, SLOT_BANKS[j][3], 0, 64)
                for j in (1, 0):
                    state_update(16 + j, SLOT_BANKS[j][3], 64, 128)
                if stop == "ctx":
                    DUMP(S, S[:].rearrange("p h v -> p (h v)"), 0, 512)
                CHK("ctx")
                run_pipelined(prep_gen, [(2 + i, xsrc[2 + i], False, i, a1fm[:, b, :], b1fm[:, b, :]) for i in range(16)])
                fw.barrier()
              CHK("prep")
              with ExitStack() as sk_:
                woutb = sb(sk_, "woutb", [128, 8, D], BF16)
                g1b = sb(sk_, "g1b", [128, D], F32); a2b = sb(sk_, "a2b", [128, D], F32); b2b = sb(sk_, "b2b", [128, D], F32)
                fw.dma("pool", lambda q: q.dma_start(out=woutb[:], in_=wout_d.rearrange("(k p) n -> p k n", p=128)), w=[woutb])
                fw.dma("sp", lambda q: q.dma_start(out=g1b[:], in_=mod_d[b:b + 1, 2 * D:3 * D].partition_broadcast(128)), r=[MODD], w=[g1b])
                fw.dma("sp", lambda q: q.dma_start(out=b2b[:], in_=nmlp_d), w=[b2b])
                fw.dma("sp", lambda q: q.dma_start(out=a2b[:], in_=mod_d[b:b + 1, 4 * D:5 * D].partition_broadcast(128)), r=[MODD], w=[a2b])
                OP("dve", lambda e: e.scalar_tensor_tensor(out=a2b[:], in0=a2b[:], scalar=1.0, in1=b2b[:], op0=ALU.add, op1=ALU.mult),
                   r=[a2b, b2b], w=[a2b])
                fw.dma("sp", lambda q: q.dma_start(out=b2b[:], in_=mod_d[b:b + 1, 3 * D:4 * D].partition_broadcast(128)), r=[MODD], w=[b2b])
                if b == 0:
                    zt = sb(sk_, "zt", [128, D], F32)
                    OP("pool", lambda e: e.memset(zt[:], 0.0), w=[zt])
                    for i_ in range(NTILE + 1):
                        zero_todo.append(lambda i_=i_: fw.dma("sp", lambda q: q.dma_start(out=yacc_d[i_ * 128:(i_ + 1) * 128, :], in_=zt[:]), r=[zt]))
                    zero_todo.append(lambda: fw.dma("sp", lambda q: q.dma_start(out=x2_d[NT:NT + 128, :], in_=zt[:, 0:512].bitcast(BF16)), r=[zt]))
                V = []
                for s_ in range(2):
                    V.append(dict(
                        xr=sb(sk_, f"xr{s_}", [128, D], F32), h1=sb(sk_, f"h1{s_}", [128, D], F32), mix=sb(sk_, f"mix{s_}", [128, D], BF16),
                        mixT=sb(sk_, f"mixT{s_}", [128, 8, 128], BF16), rawT=sb(sk_, f"rawT{s_}", [128, 512], BF16),
                        junk=sb(sk_, f"junkb{s_}", [128, D], BF16), aTf=sb(sk_, f"aTf{s_}", [128, 128], BF16), aTb=sb(sk_, f"aTb{s_}", [128, 128], BF16),
                        tv=sb(sk_, f"tvb{s_}", [128, 32], F32), lg=sb(sk_, f"lg{s_}", [128, E], F32), msk=sb(sk_, f"msk{s_}", [128, E], F32),
                        mskb=sb(sk_, f"mskb{s_}", [128, E], BF16), t8=sb(sk_, f"t8{s_}", [128, 8], F32), addr=sb(sk_, f"addr{s_}", [128, E], F32),
                        oh=sb(sk_, f"oh{s_}", [128, E], F32), dst=sb(sk_, f"dst{s_}", [128, 8], F32),
                        dsi=[sb(sk_, f"dsi{s_}{r_}", [128, 4], I32) for r_ in range(4)],
                        pk=[sb(sk_, f"pk{s_}{r_}", [128, 4, 2], I32) for r_ in range(4)], e4=sb(sk_, f"e4{s_}", [128, 4], F32)))

                def back_gen(i, slot):
                    v_ = V[slot]
                    t0, t1, t2, t3 = SLOT_BANKS[slot]
                    xr, h1, mix, mixT, rawT, junk, aTf, aTb, tv = (v_[k] for k in ("xr", "h1", "mix", "mixT", "rawT", "junk", "aTf", "aTb", "tv"))
                    lg, msk, mskb, t8, addr, oh, dst, dsi, pki, e4 = (v_[k] for k in ("lg", "msk", "mskb", "t8", "addr", "oh", "dst", "dsi", "pk", "e4"))
                    dsi, pki = dsi[(i // 2) % 4], pki[(i // 2) % 4]
                    x2, x2T = mix, mixT
                    tg = b * 16 + i
                    rows = slice(tg * 128, (tg + 1) * 128)
                    u_mm(i, t0)
                    state_update(i, t0, 64, 128, store=dSS[i])
                    fw.dma("sp", lambda q: q.dma_start(out=xr[:], in_=x_d[rows, :]), w=[xr])
                    yield
                    conv_step(pace=[xr])
                    for _ in range(3):
                        if zero_todo:
                            zero_todo.pop(0)()
                    for h in range(4):
                        OP("pe", lambda e: e.matmul(pb(t1, 0, 128), lhsT=kTS[i][0:64, h, :], rhs=qTS[i][0:64, h, :], start=True, stop=True),
                           r=[kTS[i], qTS[i]], w=[P[t1]])
                        OP("pe", lambda e: e.matmul(pb(t2, 0, 128), lhsT=kTS[i][64:128, h, :], rhs=qTS[i][64:128, h, :], start=True, stop=True),
                           r=[kTS[i], qTS[i]], w=[P[t2]])
                        OP("dve", lambda e: e.tensor_tensor(out=aTf[:], in0=pb(t1, 0, 128), in1=cb[:, CB_MF:CB_MF + 128], op=ALU.mult), r=[P[t1], cb], w=[aTf])
                        OP("dve", lambda e: e.tensor_tensor(out=aTb[:], in0=pb(t2, 0, 128), in1=cb[:, CB_MB:CB_MB + 128], op=ALU.mult), r=[P[t2], cb], w=[aTb])
                        o_ps = pb(t3, h * 128, (h + 1) * 128)
                        vh = vS[i][:, h * 128:(h + 1) * 128]
                        OP("pe", lambda e: e.matmul(o_ps, lhsT=aTf[:], rhs=vh, start=True, stop=False), r=[aTf, vS[i]], w=[P[t3]])
                        OP("pe", lambda e: e.matmul(o_ps, lhsT=aTb[:], rhs=vh, start=False, stop=False), r=[aTb, vS[i]], w=[P[t3]])
                        OP("pe", lambda e: e.matmul(o_ps, lhsT=qTS[i][:, h, :], rhs=dSS[i][:, h, :], start=False, stop=True), r=[qTS[i], dSS[i]], w=[P[t3]])
                        yield
                    for h in range(4):
                        OP("act", lambda e: e.activation(out=junk[:, h * 128:(h + 1) * 128], in_=pb(t3, h * 128, (h + 1) * 128), func=AF.Square,
                                                         accum_out=tv[:, 8 + h:9 + h]), r=[P[t3]], w=[junk, tv])
                    rstd_ss(tv, tv[:, 8:12], tv[:, 12:16], 128)
                    for h in range(4):
                        OP("dve", lambda e: e.scalar_tensor_tensor(out=mix[:, h * 128:(h + 1) * 128], in0=pb(t3, h * 128, (h + 1) * 128),
                                                                   scalar=tv[:, 12 + h:13 + h], in1=gwS[i][:, h * 128:(h + 1) * 128],
                                                                   op0=ALU.mult, op1=ALU.mult), r=[P[t3], tv, gwS[i]], w=[mix])
                    yield
                    for g in range(4):
                        js = [j for j in range(16) if (g, i, j) in _POOL_INDEX]
                        for n, j in enumerate(js):
                            m = _POOL_INDEX[(g, i, j)]
                            OP("pe", lambda e: e.matmul(pb(t0, g * 128, (g + 1) * 128), lhsT=plS[j][:, g * 128:(g + 1) * 128],
                                                        rhs=cb[:, CB_POOL + m * 128:CB_POOL + (m + 1) * 128], start=(n == 0), stop=(n == len(js) - 1)),
                               r=[plS[j], cb], w=[P[t0]])
                    OP("act", lambda e: e.activation(out=rawT[:], in_=pb(t0), func=AF.Copy), r=[P[t0]], w=[rawT])
                    yield
                    for g in range(4):
                        OP("pe", lambda e: e.matmul(pb(t1, g * 128, (g + 1) * 128), lhsT=rawT[:, g * 128:(g + 1) * 128], rhs=wpoolb[:, g, :],
                                                    start=True, stop=True), r=[rawT, wpoolb], w=[P[t1]])
                    for g in range(4):
                        OP("dve", lambda e: e.scalar_tensor_tensor(out=mix[:, 512 + g * 128:512 + (g + 1) * 128], in0=pb(t1, g * 128, (g + 1) * 128),
                                                                   scalar=cf[:, CF_INV + i * 4 + g:CF_INV + i * 4 + g + 1],
                                                                   in1=pscb[:, g * 128:(g + 1) * 128], op0=ALU.mult, op1=ALU.mult),
                           r=[P[t1], cf, pscb], w=[mix])
                    yield
                    for k in range(8):
                        OP("pe", lambda e: e.transpose(out=pbb(t2)[:, k * 128:(k + 1) * 128], in_=mix[:, k * 128:(k + 1) * 128], identity=identb),
                           r=[mix, cb], w=[P[t2]])
                    OP("act", lambda e: e.activation(out=mixT[:].rearrange("p k t -> p (k t)"), in_=pbb(t2), func=AF.Copy), r=[P[t2]], w=[mixT])
                    yield
                    for n, bk in enumerate((t0, t1)):
                        for k in range(8):
                            OP("pe", lambda e: e.matmul(pb(bk), lhsT=mixT[:, k, :], rhs=woutb[:, k, n * 512:(n + 1) * 512], start=(k == 0), stop=(k == 7)),
                               r=[mixT, woutb], w=[P[bk]])
                        OP("dve", lambda e: e.tensor_tensor(out=h1[:, n * 512:(n + 1) * 512], in0=pb(bk), in1=g1b[:, n * 512:(n + 1) * 512], op=ALU.mult),
                           r=[P[bk], g1b], w=[h1])
                    OP("dve", lambda e: e.tensor_tensor(out=h1[:], in0=h1[:], in1=xr[:], op=ALU.add), r=[h1, xr], w=[h1])
                    fw.dma("sp", lambda q: q.dma_start(out=h1_d[rows, :], in_=h1[:]), r=[h1], w=[H1D])
                    yield
                    OP("act", lambda e: e.activation(out=junk[:], in_=h1[:], func=AF.Square, accum_out=tv[:, 16:17]), r=[h1], w=[junk, tv])
                    rstd_ss(tv, tv[:, 16:17], tv[:, 17:18], D)
                    OP("dve", lambda e: e.scalar_tensor_tensor(out=xr[:], in0=h1[:], scalar=tv[:, 17:18], in1=a2b[:], op0=ALU.mult, op1=ALU.mult),
                       r=[h1, tv, a2b], w=[xr])
                    OP("dve", lambda e: e.tensor_tensor(out=x2[:], in0=xr[:], in1=b2b[:], op=ALU.add), r=[xr, b2b], w=[x2])
                    fw.dma("sp", lambda q: q.dma_start(out=x2_d[rows, :], in_=x2[:]), r=[x2], w=[X2D])
                    yield
                    for k in range(8):
                        OP("pe", lambda e: e.transpose(out=pbb(t2)[:, k * 128:(k + 1) * 128], in_=x2[:, k * 128:(k + 1) * 128], identity=identb),
                           r=[x2, cb], w=[P[t2]])
                    OP("act", lambda e: e.activation(out=x2T[:].rearrange("p k t -> p (k t)"), in_=pbb(t2), func=AF.Copy), r=[P[t2]], w=[x2T])
                    for k in range(8):
                        OP("pe", lambda e: e.matmul(pb(t3, 0, E), lhsT=x2T[:, k, :], rhs=wrb[:, k, :], start=(k == 0), stop=(k == 7)), r=[x2T, wrb], w=[P[t3]])
                    OP("dve", lambda e: e.tensor_tensor(out=lg[:], in0=pb(t3, 0, E), in1=brb[:], op=ALU.add), r=[P[t3], brb], w=[lg])
                    OP("dve", lambda e: e.max(out=t8[:], in_=lg[:]), r=[lg], w=[t8])
                    OP("dve", lambda e: e.tensor_scalar(out=msk[:], in0=lg[:], scalar1=t8[:, 3:4], scalar2=None, op0=ALU.is_ge), r=[lg, t8], w=[msk])
                    OP("dve", lambda e: e.tensor_copy(out=mskb[:], in_=msk[:]), r=[msk], w=[mskb])
                    yield
                    OP("dve", lambda e: e.tensor_scalar(out=tv[:, 20:21], in0=t8[:, 0:1], scalar1=-1.0, scalar2=None, op0=ALU.mult), r=[t8, tv], w=[tv])
                    OP("act", lambda e: e.activation(out=e4[:], in_=t8[:, 0:4], func=AF.Exp, bias=tv[:, 20:21], accum_out=tv[:, 21:22]),
                       r=[t8, tv], w=[e4, tv])
                    OP("dve", lambda e: e.reciprocal(out=tv[:, 22:23], in_=tv[:, 21:22]), r=[tv], w=[tv])
                    OP("dve", lambda e: e.tensor_scalar(out=e4[:], in0=e4[:], scalar1=tv[:, 22:23], scalar2=None, op0=ALU.mult), r=[e4, tv], w=[e4])
                    OP("pe", lambda e: e.matmul(pb(t3, 64, 64 + E), lhsT=cb[:, CB_SU:CB_SU + 128], rhs=mskb[:], start=True, stop=True), r=[cb, mskb], w=[P[t3]])
                    OP("pe", lambda e: e.matmul(pb(t3, 128, 128 + E), lhsT=cb[:, CB_ONES:CB_ONES + 128], rhs=mskb[:], start=True, stop=True), r=[cb, mskb], w=[P[t3]])
                    OP("dve", lambda e: e.tensor_tensor(out=addr[:], in0=pb(t3, 64, 64 + E), in1=carry[:], op=ALU.add), r=[P[t3], carry], w=[addr])
                    OP("dve", lambda e: e.tensor_tensor(out=carry[:], in0=pb(t3, 128, 128 + E), in1=carry[:], op=ALU.add), r=[P[t3], carry], w=[carry])
                    OP("dve", lambda e: e.tensor_tensor(out=addr[:], in0=addr[:], in1=cf[:, CF_ECAP:CF_ECAP + E], op=ALU.add), r=[addr, cf], w=[addr])
                    for k in range(4):
                        OP("dve", lambda e: e.tensor_scalar(out=oh[:], in0=lg[:], scalar1=t8[:, k:k + 1], scalar2=None, op0=ALU.is_equal), r=[lg, t8], w=[oh])
                        OP("dve", lambda e: e.tensor_tensor(out=oh[:], in0=oh[:], in1=addr[:], op=ALU.mult), r=[oh, addr], w=[oh])
                        OP("dve", lambda e: e.reduce_sum(out=dst[:, k:k + 1], in_=oh[:], axis=mybir.AxisListType.X), r=[oh], w=[dst])
                    OP("dve", lambda e: e.tensor_copy(out=dsi[:], in_=dst[:, 0:4]), r=[dst], w=[dsi])
                    OP("dve", lambda e: e.tensor_scalar(out=pki[:, :, 0], in0=z4[:], scalar1=cf[:, CF_TOK:CF_TOK + 1], scalar2=float(tg * 128),
                                                        op0=ALU.add, op1=ALU.add), r=[z4, cf], w=[pki])
                    OP("dve", lambda e: e.tensor_copy(out=pki[:, :, 1], in_=e4[:].bitcast(I32)), r=[e4], w=[pki])
                    for k in range(4):
                        fw.dma("pool", lambda q: q.indirect_dma_start(out=lists_d, out_offset=bass.IndirectOffsetOnAxis(ap=dsi[:, k:k + 1], axis=0),
                                                                      in_=pki[:, k, :], in_offset=None), r=[pki, dsi])

                run_pipelined(back_gen, list(range(15, -1, -1)))
                while zero_todo:
                    zero_todo.pop(0)()
                fw.barrier()
            nbf = sb(st, "nbf", [128, 2 * E], F32)
            OP("pool", lambda e: e.memset(nbf[:], 0.0), w=[nbf])
            for j in range(NBMAX):
                OP("dve", lambda e: e.scalar_tensor_tensor(out=nbf[:, 0:E], in0=carry[:], scalar=float(128 * j), in1=nbf[:, 0:E],
                                                           op0=ALU.is_gt, op1=ALU.add), r=[carry, nbf], w=[nbf])
            OP("dve", lambda e: e.tensor_copy(out=nbi[:], in_=nbf[:, 0:E]), r=[nbf], w=[nbi])
            fw.dma("sp", lambda q: q.dma_start(out=nblk_d, in_=nbi[0:1, :]), r=[nbi], w=[NBLKD])
            fw.barrier()

        if phases >= 3:
          with ExitStack() as st:
            wgu = [sb(st, f"wgu{i}", [128, 8, 2 * D], BF16) for i in range(2)]
            wdn = [sb(st, f"wdn{i}", [128, 8, D], BF16) for i in range(2)]
            bst = sb(st, "bst", [1, 3 * D], F32)
            ids = [sb(st, f"ids{i}", [128, 2], I32) for i in range(3)]
            xg = [sb(st, f"xg{i}", [128, D], BF16) for i in range(3)]
            xgT = [sb(st, f"xgT{i}", [128, 8, 128], BF16) for i in range(2)]
            gc = [[sb(st, f"gc{p}{h}", [128, 512], F32) for h in range(2)] for p in range(1)]
            sg = [[sb(st, f"sg{p}{h}", [128, 512], F32) for h in range(2)] for p in range(1)]
            uc = [[sb(st, f"uc{p}{h}", [128, 512], F32) for h in range(2)] for p in range(1)]
            actb = [[sb(st, f"actb{p}{h}", [128, 512], BF16) for h in range(2)] for p in range(1)]
            actT = [[sb(st, f"actT{p}{h}", [128, 4, 128], BF16) for h in range(2)] for p in range(1)]
            yw = [sb(st, f"yw{i}", [128, D], F32) for i in range(2)]
            ones1b = cb[0:1, CB_ONES:CB_ONES + 128]
            bhl = [[sb(st, f"bhl{i}{j}", [1, 3 * D], BF16) for j in range(1)] for i in range(2)]
            regs = nc.alloc_registers("nblk", [ET.PE, ET.Activation, ET.DVE, ET.Pool, ET.SP])
            PGh = [T(None), T(None)]
            PD = T(None); PT0 = P[0]
            PT1h = [T(None), T(None)]

            def load_bias(ex):
                fw.dma("sp", lambda q: q.dma_start(out=bst[0:1, 0:2 * D], in_=bgu_d[ex:ex + 1, :]), w=[bst])
                fw.dma("sp", lambda q: q.dma_start(out=bst[0:1, 2 * D:3 * D], in_=bdn_d[ex:ex + 1, :]), w=[bst])
                hi = bhl[ex % 2][0]
                OP("dve", lambda e: e.tensor_copy(out=hi[:], in_=bst[:]), r=[bst], w=[hi])

            wgh = [[T(None) for _ in range(8)] for _ in range(2)]
            wdh = [[T(None) for _ in range(8)] for _ in range(2)]

            def weight_pieces(ex):
                wg, wd = wgu[ex % 2], wdn[ex % 2]
                pcs = []
                for k in range(8):
                    pcs.append(lambda k=k: fw.dma("sp", lambda q: q.dma_start(out=wg[:, k, :], in_=wgu16_d[ex, k * 128:(k + 1) * 128, :]),
                                                  r=[WCV[ex][0]], w=[wgh[ex % 2][k]]))
                for k in range(8):
                    pcs.append(lambda k=k: fw.dma("sp", lambda q: q.dma_start(out=wd[:, k, :], in_=wdn16_d[ex, k * 128:(k + 1) * 128, :]),
                                                  r=[WCV[ex][1]], w=[wdh[ex % 2][k]]))
                return pcs

            ids0 = [sb(st, f"ids0{i}", [128, 2], I32) for i in range(2)]
            xg0 = [sb(st, f"xg0{i}", [128, D], BF16) for i in range(2)]

            def blk_bufs(ex, j):
                nbidx = ex * nbmax + j
                return (ids0[ex % 2], xg0[ex % 2]) if j == 0 else (ids[nbidx % 3], xg[nbidx % 3])

            def issue_fetch(ex, j):
                idt, xgt = blk_bufs(ex, j)
                blk = ex * NBMAX + j
                fw.dma("pool", lambda q: q.dma_start(out=idt[:], in_=lists_d[blk * 128:(blk + 1) * 128, :]), r=[LISTS], w=[idt])
                fw.dma("pool", lambda q: q.indirect_dma_start(out=xgt[:], out_offset=None, in_=x2_d,
                                                              in_offset=bass.IndirectOffsetOnAxis(ap=idt[:, 0:1], axis=0)), r=[X2D, idt], w=[xgt])

            def emit_tx(ex, j):
                nbidx = ex * nbmax + j
                xgt, xT = blk_bufs(ex, j)[1], xgT[nbidx % 2]
                for k in range(8):
                    OP("pe", lambda e: e.transpose(out=pbb(0)[:, k * 128:(k + 1) * 128], in_=xgt[:, k * 128:(k + 1) * 128], identity=identb),
                       r=[xgt, cb], w=[PT0])
                OP("act", lambda e: e.activation(out=xT[:].rearrange("p k t -> p (k t)"), in_=pbb(0), func=AF.Copy), r=[PT0], w=[xT])

            while conv_todo:
                conv_step()
            load_bias(0)
            issue_fetch(0, 0)
            for pc in weight_pieces(0):
                pc()
            for ex in range(E):
                pcs = []
                if ex + 1 < E:
                    issue_fetch(ex + 1, 0)
                    load_bias(ex + 1)
                    pcs = weight_pieces(ex + 1)
                wg, wd = wgu[ex % 2], wdn[ex % 2]
                bhi = bhl[ex % 2][0]
                for e_ in fw.ENG:
                    fw.sync_to(e_, [nbi])
                for reg in regs:
                    nc.reg_load(reg, nbi[0:1, ex:ex + 1])

                def emit_block(j):
                    fw.begin_if_cmp(regs, j, "IS_GT")
                    nbidx = ex * nbmax + j
                    (idt, xgt), ywt = blk_bufs(ex, j), yw[nbidx % 2]
                    par = 0
                    xT = xgT[nbidx % 2]
                    if j + 1 < nbmax:
                        issue_fetch(ex, j + 1)
                    emit_tx(ex, j)
                    for hf in range(2):
                        for gi, c0 in enumerate((hf * 512, D + hf * 512)):
                            bk = 2 + 2 * hf + gi
                            for k in range(8):
                                OP("pe", lambda e: e.matmul(pb(bk), lhsT=xT[:, k, :], rhs=wg[:, k, c0:c0 + 512], start=(k == 0), stop=False),
                                   r=[xT, wgh[ex % 2][k]], w=[PGh[hf]])
                            OP("pe", lambda e: e.matmul(pb(bk), lhsT=ones1b, rhs=bhi[0:1, c0:c0 + 512], start=False, stop=True), r=[cb, bhi], w=[PGh[hf]])
                    for hf in range(2):
                        g_, s_, u_, a_, aT_ = gc[par][hf], sg[par][hf], uc[par][hf], actb[par][hf], actT[par][hf]
                        OP("dve", lambda e: e.tensor_scalar(out=g_[:], in0=pb(2 + 2 * hf), scalar1=7.0, scalar2=None, op0=ALU.min), r=[PGh[hf]], w=[g_])
                        OP("act", lambda e: e.activation(out=s_[:], in_=g_[:], func=AF.Sigmoid, scale=1.702), r=[g_], w=[s_])
                        OP("dve", lambda e: e.tensor_scalar(out=u_[:], in0=pb(3 + 2 * hf), scalar1=-7.0, scalar2=7.0, op0=ALU.max, op1=ALU.min), r=[PGh[hf]], w=[u_])
                        OP("dve", lambda e: e.scalar_tensor_tensor(out=u_[:], in0=u_[:], scalar=1.0, in1=g_[:], op0=ALU.add, op1=ALU.mult), r=[u_, g_], w=[u_])
                        OP("dve", lambda e: e.tensor_tensor(out=a_[:], in0=u_[:], in1=s_[:], op=ALU.mult), r=[u_, s_], w=[a_])
                        for c in range(4):
                            OP("pe", lambda e: e.transpose(out=pbb(1)[:, (hf * 4 + c) * 128:(hf * 4 + c + 1) * 128], in_=a_[:, c * 128:(c + 1) * 128], identity=identb),
                               r=[a_, cb], w=[PT1h[hf]])
                        OP("act", lambda e: e.activation(out=aT_[:].rearrange("p k t -> p (k t)"), in_=pbb(1)[:, hf * 512:(hf + 1) * 512], func=AF.Copy),
                           r=[PT1h[hf]], w=[aT_])
                        for n in range(2):
                            for k in range(4 * hf, 4 * hf + 4):
                                OP("pe", lambda e: e.matmul(pb(6 + n), lhsT=aT_[:, k % 4, :], rhs=wd[:, k, n * 512:(n + 1) * 512], start=(k == 0), stop=False),
                                   r=[aT_, wdh[ex % 2][k]], w=[PD])
                    for n in range(2):
                        OP("pe", lambda e: e.matmul(pb(6 + n), lhsT=ones1b, rhs=bhi[0:1, 2 * D + n * 512:2 * D + (n + 1) * 512], start=False, stop=True), r=[cb, bhi], w=[PD])
                    OP("act", lambda e: e.activation(out=ywt[:], in_=psum[:, 6 * 512:8 * 512], func=AF.Copy, scale=idt[:, 1:2].bitcast(F32)), r=[PD, idt], w=[ywt])
                    fw.dma("pool", lambda q: q.indirect_dma_start(out=yacc_d, out_offset=bass.IndirectOffsetOnAxis(ap=idt[:, 0:1], axis=0),
                                                                  in_=ywt[:], in_offset=None, compute_op=ALU.add), r=[ywt, idt, YACC], w=[YACC])

                while pcs:
                    pcs.pop(0)()
                for j in range(nbmax):
                    emit_block(j)
                for j in range(nbmax):
                    fw.end_if()
            fw.barrier()

        if phases >= 4:
          with ExitStack() as st:
            g2b = [sb(st, f"g2b{i}", [128, D], F32) for i in range(2)]
            fnwb = sb(st, "fnwb", [128, D], F32)
            NF = 4
            F_ = [dict(h=sb(st, f"fh{i}", [128, D], F32), y=sb(st, f"fy{i}", [128, D], F32), o=sb(st, f"fo{i}", [128, D], F32),
                       junk=sb(st, f"fj{i}", [128, D], BF16), tv=sb(st, f"ftv{i}", [128, 4], F32), out=T(None)) for i in range(NF)]
            fw.dma("sp", lambda q: q.dma_start(out=fnwb[:], in_=fnw_d), w=[fnwb])
            for b in range(2):
                fw.dma("sp", lambda q: q.dma_start(out=g2b[b][:], in_=mod_d[b:b + 1, 5 * D:6 * D].partition_broadcast(128)), r=[MODD], w=[g2b[b]])

            def final_gen(tg, slot):
                f_ = F_[slot]
                h_, y_, o_, jk, tv, OUT = f_["h"], f_["y"], f_["o"], f_["junk"], f_["tv"], f_["out"]
                rows = slice(tg * 128, (tg + 1) * 128)
                fw.dma("sp", lambda q: q.dma_start(out=h_[:], in_=h1_d[rows, :]), r=[H1D], w=[h_])
                fw.dma("sp", lambda q: q.dma_start(out=y_[:], in_=yacc_d[rows, :]), r=[YACC], w=[y_])
                yield
                OP("dve", lambda e: e.tensor_tensor(out=y_[:], in0=y_[:], in1=g2b[tg // 16][:], op=ALU.mult), r=[y_, g2b[tg // 16]], w=[y_])
                OP("dve", lambda e: e.tensor_tensor(out=h_[:], in0=h_[:], in1=y_[:], op=ALU.add), r=[h_, y_], w=[h_])
                OP("act", lambda e: e.activation(out=jk[:], in_=h_[:], func=AF.Square, accum_out=tv[:, 0:1]), r=[h_], w=[jk, tv])
                yield
                rstd_ss(tv, tv[:, 0:1], tv[:, 1:2], D)
                yield
                OP("dve", lambda e: e.scalar_tensor_tensor(out=o_[:], in0=h_[:], scalar=tv[:, 1:2], in1=fnwb[:], op0=ALU.mult, op1=ALU.mult),
                   r=[h_, tv, fnwb], w=[o_])
                fw.dma("sp", lambda q: q.dma_start(out=out_d[rows, :], in_=o_[:]), r=[o_], w=[OUT])

            run_pipelined(final_gen, list(range(NTILE)), nslots=NF, gap=1)
            fw.barrier()

    except _Stop:
        fw.barrier()
        return nc, fw
    root.close()
    return nc, fw


_WIN_PERM = np.concatenate([np.arange(0, 1536), np.arange(1568, 2080), np.arange(1536, 1568)])


def make_in_maps(x, c, ctx, c_ctx, w_ada, b_ada, norm_mix_w, norm_mlp_w, w_in, w_gk_f, b_gk_f, w_gk_b, b_gk_b,
                 gla_norm_w, w_pool, pool_scale, w_out, w_router, b_router, w_gu, b_gu, w_down, b_down, final_norm_w):
    f = lambda a: np.ascontiguousarray(np.asarray(a, dtype=np.float32))
    cf, cbf = _consts()
    wgk = np.zeros((32, 4, 2, 64), np.float32)
    wgk[0:16, :, 0, :] = f(w_gk_f)[0].reshape(16, 4, 64)
    wgk[16:32, :, 1, :] = f(w_gk_b)[0].reshape(16, 4, 64)
    bgk = np.stack([f(b_gk_f)[0].reshape(4, 64), f(b_gk_b)[0].reshape(4, 64)], axis=1).reshape(1, 512)
    rep = lambda v: np.ascontiguousarray(np.broadcast_to(f(v).reshape(1, -1), (128, f(v).size)))
    shared = {
        "w_ada": f(w_ada)[0], "b_ada": f(b_ada)[0].reshape(1, -1),
        "nmwT": np.ascontiguousarray(f(norm_mix_w)[0].reshape(8, 128).T), "nmlp_bc": rep(norm_mlp_w), "fnw_bc": rep(final_norm_w),
        "w_in": np.ascontiguousarray(f(w_in)[0][:, _WIN_PERM]), "wgk": wgk.reshape(32, 512), "bgk": np.ascontiguousarray(bgk),
        "gnw_bc": rep(np.tile(f(gla_norm_w)[0], 4)), "w_pool": f(w_pool)[0], "psc_bc": rep(pool_scale),
        "w_out": f(w_out)[0], "w_router": f(w_router)[0], "br_bc": rep(b_router),
        "w_gu": f(w_gu)[0], "b_gu": f(b_gu)[0], "w_down": f(w_down)[0], "b_down": f(b_down)[0],
        "cf": cf, "cb": cbf, "linit": _list_init(),
    }
    x = f(x); ctx = f(ctx); c = f(c); c_ctx = f(c_ctx)
    maps = []
    for i in range(NCORES):
        cv = np.stack([c[2 * i], c[2 * i + 1], c_ctx], axis=0)
        cT = np.ascontiguousarray(cv.reshape(3, 8, 128).transpose(2, 1, 0))
        m = dict(shared)
        m["x"] = x[2 * i:2 * i + 2].reshape(NT, D)
        m["ctx"] = ctx[2 * i:2 * i + 2].reshape(NCTX, D)
        m["cT"] = cT
        maps.append(m)
    return maps


def kernel(**inputs):
    nc, _ = build_program()
    maps = make_in_maps(**inputs)
    res = run_bass_kernel_spmd(nc, maps, core_ids=list(range(NCORES)))
    out = np.stack([np.asarray(r["out"]).reshape(2, 2048, D) for r in res.results], axis=0)
    return out.reshape(16, 2048, D).astype(np.float32)
```

```python
import numpy as np
import concourse.bass as bass
import concourse.mybir as mybir
from concourse.bass_utils import run_bass_kernel_spmd

F32 = mybir.dt.float32
BF16 = mybir.dt.bfloat16
I32 = mybir.dt.int32
ALU = mybir.AluOpType
AF = mybir.ActivationFunctionType
ET = mybir.EngineType

NCORES = 8
D = 1024
NT = 4096
NTILE = 32
NCTX = 512
E = 32
NBMAX = 32
CAP = NBMAX * 128
EPS = 1e-6
POOL_W = (2, 4, 8, 16)


class Buf:
    __slots__ = ("w", "r")

    def __init__(self):
        self.w = None
        self.r = {}


class T:
    def __init__(self, t):
        self.t = t
        self.b = Buf()

    def __getitem__(self, k):
        return self.t[k]


class FW:
    ENG = ("pe", "act", "dve", "pool", "sp")

    def __init__(self, nc, n_dma_sems=(14, 10)):
        self.nc = nc
        self.eng = {"pe": nc.tensor, "act": nc.scalar, "dve": nc.vector, "pool": nc.gpsimd, "sp": nc.sync}
        self.sem = {k: nc.alloc_semaphore(f"s_{k}") for k in self.ENG}
        self.cnt = {k: 0 for k in self.ENG}
        tot = sum(n_dma_sems)
        self.dsem = [nc.alloc_semaphore(f"d_{i}") for i in range(tot)]
        self.dcnt = [0] * tot
        self.dpool = {}
        self.downer = {}
        o = 0
        for q, n in zip(("sp", "pool"), n_dma_sems):
            self.dpool[q] = [list(range(o, o + n)), 0]
            for i in range(o, o + n):
                self.downer[i] = q
            o += n
        self.seen = {k: {} for k in self.ENG}
        self.if_stack = []
        self.n_ins = 0

    def _semh(self, key):
        return self.sem[key] if isinstance(key, str) else self.dsem[key]

    def _deps(self, reads, writes):
        deps = {}

        def add(k, v):
            if deps.get(k, 0) < v:
                deps[k] = v
        for t in reads:
            if t.b.w is not None:
                add(*t.b.w)
        for t in writes:
            if t.b.w is not None:
                add(*t.b.w)
            for k, v in t.b.r.items():
                add(k, v)
        return deps

    def _need(self, e, deps, skip_self=False):
        need = []
        for k, v in deps.items():
            if skip_self and k == e:
                continue
            if self.seen[e].get(k, 0) < v:
                need.append((k, v))
                self.seen[e][k] = v
        return need

    def op(self, e, fn, r=(), w=()):
        need = self._need(e, self._deps(r, w), skip_self=(e == "pe"))
        eng = self.eng[e]
        for k, v in need[1:]:
            eng.wait_ge(self._semh(k), v)
        ins = fn(eng)
        if need:
            ins._wait_ge(self._semh(need[0][0]), need[0][1])
        self.cnt[e] += 1
        ins.then_inc(self.sem[e], 1)
        c = self.cnt[e]
        for t in w:
            t.b.w = (e, c)
            t.b.r = {}
        for t in r:
            if t.b.r.get(e, 0) < c:
                t.b.r[e] = c
        self.n_ins += 1
        return ins

    def dma(self, q, fn, r=(), w=()):
        deps = self._deps(r, w)
        pl = self.dpool[q]
        si = pl[0][pl[1]]
        pl[1] = (pl[1] + 1) % len(pl[0])
        if self.dcnt[si] > 0:
            deps[si] = max(deps.get(si, 0), self.dcnt[si])
        need = self._need(q, deps)
        eng = self.eng[q]
        for k, v in need[1:]:
            eng.wait_ge(self._semh(k), v)
        ins = fn(eng)
        if need:
            ins._wait_ge(self._semh(need[0][0]), need[0][1])
        self.dcnt[si] += 16
        ins.then_inc(self.dsem[si], 16)
        v = self.dcnt[si]
        for t in w:
            t.b.w = (si, v)
            t.b.r = {}
        for t in r:
            t.b.r[si] = v
        self.n_ins += 1
        return ins

    def sync_to(self, e, reads):
        for k, v in self._need(e, self._deps(reads, ())):
            self.eng[e].wait_ge(self._semh(k), v)

    def wait_all(self, e):
        eng = self.eng[e]
        for k in self.ENG:
            if k != e and self.cnt[k] > self.seen[e].get(k, 0):
                eng.wait_ge(self.sem[k], self.cnt[k])
                self.seen[e][k] = self.cnt[k]
        for i, v in enumerate(self.dcnt):
            if v > self.seen[e].get(i, 0):
                eng.wait_ge(self.dsem[i], v)
                self.seen[e][i] = v

    def barrier(self):
        for e in self.ENG:
            self.wait_all(e)

    def begin_if_cmp(self, regs, val, op):
        st = dict(cnt=dict(self.cnt), dcnt=list(self.dcnt), seen={k: dict(v) for k, v in self.seen.items()})
        g = self.nc.If_cmp(regs, val, op)
        g.__enter__()
        st["g"] = g
        self.if_stack.append(st)

    def end_if(self):
        st = self.if_stack.pop()
        st["g"].__exit__(None, None, None)
        g = self.nc.Else()
        g.__enter__()
        for e in self.ENG:
            eng = self.eng[e]
            d = self.cnt[e] - st["cnt"][e]
            if d > 0:
                eng.wait_ge(self.sem[e], st["cnt"][e])
                eng.sem_inc(self.sem[e], d)
            for si in range(len(self.dsem)):
                dd = self.dcnt[si] - st["dcnt"][si]
                if dd > 0 and self.downer[si] == e:
                    eng.wait_ge(self.dsem[si], st["dcnt"][si])
                    eng.sem_inc(self.dsem[si], dd)
        g.__exit__(None, None, None)
        self.seen = st["seen"]


def _window(pos, w, length):
    lo = w // 2
    hi = w - 1 - lo
    return np.clip(pos - lo, 0, length), np.clip(pos + hi + 1, 0, length)


def _pool_consts():
    mats, index, keymap = [], {}, {}
    invcnt = np.zeros((128, 64), np.float32)
    p = np.arange(128)
    for g, w in enumerate(POOL_W):
        lo = w // 2
        hi = w - 1 - lo
        for i in range(16):
            rt = 2 * i + p // 64
            ct = p % 64
            r0, r1 = _window(rt, w, 32)
            c0, c1 = _window(ct, w, 64)
            cnt = ((r1 - r0) * (c1 - c0)).astype(np.float32)
            invcnt[:, i * 4 + g] = 1.0 / cnt
            for j in range(16):
                rs = 2 * j + p // 64
                cs = p % 64
                m = ((rs[:, None] >= rt[None, :] - lo) & (rs[:, None] <= rt[None, :] + hi) &
                     (cs[:, None] >= ct[None, :] - lo) & (cs[:, None] <= ct[None, :] + hi)).astype(np.float32)
                if not m.any():
                    continue
                if i == j:
                    m = m - np.diag(cnt)
                key = m.tobytes()
                if key not in keymap:
                    keymap[key] = len(mats)
                    mats.append(m)
                index[(g, i, j)] = keymap[key]
    return mats, index, invcnt


_POOL_MATS, _POOL_INDEX, _INVCNT = _pool_consts()
NPM = len(_POOL_MATS)

CF_ID, CF_M1F, CF_M1B, CF_INV, CF_NEG, CF_ONES, CF_SENT, CF_ECAP, CF_TOK = 0, 128, 256, 384, 448, 449, 577, 578, 610
CF_N = 611
CB_ID, CB_MF, CB_MB, CB_SU, CB_ONES, CB_POOL = 0, 128, 256, 384, 512, 640
CB_N = CB_POOL + 128 * NPM


def _consts():
    import ml_dtypes
    s = np.arange(128)[:, None]
    t = np.arange(128)[None, :]
    cf = np.zeros((128, CF_N), np.float32)
    cf[:, CF_ID:CF_ID + 128] = np.eye(128)
    cf[:, CF_M1F:CF_M1F + 128] = (s > t) / 16.0
    cf[:, CF_M1B:CF_M1B + 128] = (s < t) / 16.0
    cf[:, CF_INV:CF_INV + 64] = _INVCNT
    cf[:, CF_NEG] = -1.0 / 16.0
    cf[:, CF_ONES:CF_ONES + 128] = 1.0
    cf[:, CF_SENT] = NT + np.arange(128)
    cf[:, CF_ECAP:CF_ECAP + 32] = (np.arange(32) * CAP)[None, :]
    cf[:, CF_TOK] = np.arange(128)
    cb = np.zeros((128, CB_N), np.float32)
    cb[:, CB_ID:CB_ID + 128] = np.eye(128)
    cb[:, CB_MF:CB_MF + 128] = (s <= t)
    cb[:, CB_MB:CB_MB + 128] = (s >= t)
    cb[:, CB_SU:CB_SU + 128] = (s < t)
    cb[:, CB_ONES:CB_ONES + 128] = 1.0
    for m, mat in enumerate(_POOL_MATS):
        cb[:, CB_POOL + 128 * m:CB_POOL + 128 * (m + 1)] = mat
    return cf, cb.astype(ml_dtypes.bfloat16)


def _list_init():
    r = np.arange(128 * E * NBMAX)
    img = np.stack([NT + r % 128, np.zeros_like(r)], axis=1).astype(np.int32)
    return np.ascontiguousarray(img.reshape(128, E * NBMAX * 2))


class _Stop(Exception):
    pass


def build_program(nbmax=NBMAX, phases=4, stop=None, debug=False):
    from contextlib import ExitStack

    def CHK(name):
        if stop == name:
            raise _Stop()

    nc = bass.Bass("TRN2", target_bir_lowering=False)
    fw = FW(nc)
    OP = fw.op

    def din(name, shape, dt=F32):
        return nc.dram_tensor(name, list(shape), dt, kind="ExternalInput").ap()

    x_d = din("x", [NT, D]); ctx_d = din("ctx", [NCTX, D]); cT_d = din("cT", [128, 8, 3])
    wada_d = din("w_ada", [D, 6 * D]); bada_d = din("b_ada", [1, 6 * D])
    nmw_d = din("nmwT", [128, 8]); nmlp_d = din("nmlp_bc", [128, D]); fnw_d = din("fnw_bc", [128, D])
    win_d = din("w_in", [D, 2080]); wgk_d = din("wgk", [32, 512]); bgk_d = din("bgk", [1, 512])
    gnw_d = din("gnw_bc", [128, 512]); wpool_d = din("w_pool", [4, 128, 128]); psc_d = din("psc_bc", [128, 512])
    wout_d = din("w_out", [D, D]); wr_d = din("w_router", [D, E]); br_d = din("br_bc", [128, E])
    wgu_d = din("w_gu", [E, D, 2 * D]); bgu_d = din("b_gu", [E, 2 * D]); wdn_d = din("w_down", [E, D, D]); bdn_d = din("b_down", [E, D])
    cf_d = din("cf", [128, CF_N]); cb_d = din("cb", [128, CB_N], BF16)
    linit_d = din("linit", [128, E * NBMAX * 2], I32)
    out_d = nc.dram_tensor("out", [NT, D], F32, kind="ExternalOutput").ap()
    SK = "ExternalOutput" if debug else "Internal"
    h1_d = nc.dram_tensor("h1s", [NT, D], F32, kind=SK).ap()
    x2_d = nc.dram_tensor("x2s", [NT + 128, D], BF16, kind=SK).ap()
    yacc_d = nc.dram_tensor("yacc", [NT + 128, D], F32, kind=SK).ap()
    lists_d = nc.dram_tensor("lists", [128 * E * NBMAX, 2], I32, kind=SK).ap()
    nblk_d = nc.dram_tensor("nblk", [1, E], I32, kind=SK).ap()
    mod_d = nc.dram_tensor("mods", [3, 6 * D], F32, kind=SK).ap()
    H1D, X2D, YACC, LISTS, NBLKD, MODD = (T(None) for _ in range(6))
    wgu16_d = nc.dram_tensor("wgu16", [E, D, 2 * D], BF16).ap()
    wdn16_d = nc.dram_tensor("wdn16", [E, D, D], BF16).ap()
    WCV = [[T(None), T(None)] for _ in range(E)]
    conv_todo = [(e_, h_) for e_ in range(E) for h_ in range(2)]

    def conv_step(pace=None):
        if not conv_todo:
            return
        e_, h_ = conv_todo.pop(0)
        if pace is not None:
            fw.sync_to("pool", pace)
        if h_ == 0:
            fw.dma("pool", lambda q: q.dma_start(out=wgu16_d[e_], in_=wgu_d[e_]), w=[WCV[e_][0]])
        else:
            fw.dma("pool", lambda q: q.dma_start(out=wdn16_d[e_], in_=wdn_d[e_]), w=[WCV[e_][1]])
    dbg_d = nc.dram_tensor("dbg", [128, 8192], F32, kind=SK).ap()
    DBG = T(None)

    def DUMP(t, ap, off, n):
        fw.dma("sp", lambda q: q.dma_start(out=dbg_d[0:ap.shape[0], off:off + n], in_=ap), r=[t], w=[DBG])

    _uid = [0]

    def sb(st, name, shape, dt):
        _uid[0] += 1
        return T(st.enter_context(nc.sbuf_tensor(f"sb_{name}_{_uid[0]}", list(shape), dt)))

    root = ExitStack()
    psum = nc.alloc_psum_tensor("psum", [128, 4096], F32)
    P = [T(None) for _ in range(8)]

    def pb(i, lo=0, hi=512):
        return psum[:, i * 512 + lo:i * 512 + hi]

    def pbb(i):
        return psum[:, i * 512:(i + 1) * 512].bitcast(BF16)

    cf = sb(root, "cf", [128, CF_N], F32); cb = sb(root, "cb", [128, CB_N], BF16)
    fw.dma("sp", lambda q: q.dma_start(out=cf[:], in_=cf_d), w=[cf])
    fw.dma("sp", lambda q: q.dma_start(out=cb[:], in_=cb_d), w=[cb])
    ident = cf[:, CF_ID:CF_ID + 128]
    wi16_d = nc.dram_tensor("win16", [D, 2080], BF16).ap()
    wo16_d = nc.dram_tensor("wout16", [D, D], BF16).ap()
    WI16, WO16 = T(None), T(None)
    fw.dma("pool", lambda q: q.dma_start(out=wi16_d.rearrange("d (t n) -> (d t) n", n=1040), in_=win_d.rearrange("d (t n) -> (d t) n", n=1040)), w=[WI16])
    fw.dma("pool", lambda q: q.dma_start(out=wo16_d, in_=wout_d), w=[WO16])
    identb = cb[:, CB_ID:CB_ID + 128]
    a1fm = sb(root, "a1fm", [128, 3, 8], F32); b1fm = sb(root, "b1fm", [128, 3, 8], F32)
    carry = sb(root, "carry", [128, E], F32)
    OP("pool", lambda e: e.memset(carry[:], 0.0), w=[carry])

    def rstd_from_ss(ss, rs, n):
        OP("dve", lambda e: e.tensor_scalar(out=rs, in0=ss, scalar1=1.0 / n, scalar2=EPS, op0=ALU.mult, op1=ALU.add), r=[tmpv], w=[tmpv])
        OP("act", lambda e: e.activation(out=rs, in_=rs, func=AF.Sqrt), r=[tmpv], w=[tmpv])
        OP("dve", lambda e: e.reciprocal(out=rs, in_=rs), r=[tmpv], w=[tmpv])

    tmpv = sb(root, "tmpv", [128, 64], F32)
    z4 = sb(root, "z4", [128, 4], F32)
    nbi = sb(root, "nbi", [128, E], I32)
    OP("pool", lambda e: e.memset(z4[:], 0.0), w=[z4])

    try:
        with ExitStack() as st:
            cTs = sb(st, "cTs", [128, 8, 3], F32)
            wa = [sb(st, f"wa{i}", [128, 8, 512], F32) for i in range(2)]
            badab = sb(st, "badab", [3, 6 * D], F32)
            modrow = sb(st, "modrow", [3, 6 * D], F32)
            sfm = sb(st, "sfm", [128, 3, 8], F32)
            nmw = sb(st, "nmw", [128, 8], F32)
            fw.dma("sp", lambda q: q.dma_start(out=cTs[:], in_=cT_d), w=[cTs])
            fw.dma("sp", lambda q: q.dma_start(out=badab[:], in_=bada_d.partition_broadcast(3)), w=[badab])
            fw.dma("sp", lambda q: q.dma_start(out=nmw[:], in_=nmw_d), w=[nmw])
            OP("act", lambda e: e.activation(out=cTs[:], in_=cTs[:], func=AF.Silu), r=[cTs], w=[cTs])
            wav = wada_d.rearrange("(k p) n -> p k n", p=128)
            for n in range(12):
                wt = wa[n % 2]
                fw.dma("sp", lambda q: q.dma_start(out=wt[:], in_=wav[:, :, n * 512:(n + 1) * 512]), w=[wt])
                for k in range(8):
                    OP("pe", lambda e: e.matmul(pb(0)[0:3, :], lhsT=cTs[:, k, :], rhs=wt[:, k, :], start=(k == 0), stop=(k == 7)),
                       r=[cTs, wt], w=[P[0]])
                OP("dve", lambda e: e.tensor_tensor(out=modrow[0:3, n * 512:(n + 1) * 512], in0=pb(0)[0:3, :],
                                                    in1=badab[0:3, n * 512:(n + 1) * 512], op=ALU.add), r=[P[0], badab], w=[modrow])
            fw.dma("sp", lambda q: q.dma_start(out=mod_d, in_=modrow[0:3, :]), r=[modrow], w=[MODD])
            for j in range(3):
                fw.dma("sp", lambda q: q.dma_start(out=b1fm[:, j, :], in_=mod_d[j, 0:D].rearrange("(c p) -> p c", p=128),
                                                   allow_slow_non_contiguous=True), r=[MODD], w=[b1fm])
                fw.dma("sp", lambda q: q.dma_start(out=sfm[:, j, :], in_=mod_d[j, D:2 * D].rearrange("(c p) -> p c", p=128),
                                                   allow_slow_non_contiguous=True), r=[MODD], w=[sfm])
                OP("dve", lambda e: e.scalar_tensor_tensor(out=a1fm[:, j, :], in0=sfm[:, j, :], scalar=1.0, in1=nmw[:],
                                                           op0=ALU.add, op1=ALU.mult), r=[sfm, nmw], w=[a1fm])
            fw.barrier()
        CHK("p0")

        with ExitStack() as st:
            wpoolb = sb(st, "wpoolb", [128, 4, 128], BF16)
            wrb = sb(st, "wrb", [128, 8, E], BF16)
            wgk = sb(st, "wgk", [32, 512], F32); bgk = sb(st, "bgk", [1, 512], F32)
            gnwb = sb(st, "gnwb", [128, 512], F32); pscb = sb(st, "pscb", [128, 512], F32)
            brb = sb(st, "brb", [128, E], F32)
            fw.dma("pool", lambda q: q.dma_start(out=wpoolb[:], in_=wpool_d.rearrange("g c d -> c g d")), w=[wpoolb])
            fw.dma("pool", lambda q: q.dma_start(out=wrb[:], in_=wr_d.rearrange("(k p) n -> p k n", p=128)), w=[wrb])
            for tt, dd in ((wgk, wgk_d), (bgk, bgk_d), (gnwb, gnw_d), (pscb, psc_d), (brb, br_d)):
                fw.dma("sp", lambda q: q.dma_start(out=tt[:], in_=dd), w=[tt])
            fw.dma("sp", lambda q: q.dma_start(out=lists_d.rearrange("(q r) two -> q (r two)", q=128), in_=linit_d), w=[LISTS])
            zero_todo = []
            CHK("init")

            vS = [sb(st, f"vS{i}", [128, 512], BF16) for i in range(18)]
            kdS = [sb(st, f"kdS{i}", [128, 512], BF16) for i in range(18)]
            decS = [sb(st, f"decS{i}", [128, 4], F32) for i in range(18)]
            gwS = [sb(st, f"gwS{i}", [128, 512], BF16) for i in range(16)]
            plS = [sb(st, f"plS{i}", [128, 512], BF16) for i in range(16)]
            qTS = [sb(st, f"qTS{i}", [128, 4, 128], BF16) for i in range(16)]
            kTS = [sb(st, f"kTS{i}", [128, 4, 128], BF16) for i in range(16)]
            dSS = [sb(st, f"dSS{i}", [128, 4, 128], BF16) for i in range(16)]
            S = sb(st, "S", [128, 4, 128], F32)
            ones1 = cf[0:1, CF_ONES:CF_ONES + 128]
            SLOT_BANKS = ((0, 2, 4, 6), (1, 3, 5, 7))

            def rstd_ss(tv, ss, rs, n):
                OP("dve", lambda e: e.tensor_scalar(out=rs, in0=ss, scalar1=1.0 / n, scalar2=EPS, op0=ALU.mult, op1=ALU.add), r=[tv], w=[tv])
                OP("act", lambda e: e.activation(out=rs, in_=rs, func=AF.Sqrt), r=[tv], w=[tv])
                OP("dve", lambda e: e.reciprocal(out=rs, in_=rs), r=[tv], w=[tv])

            def run_pipelined(make_gen, items, nslots=2, gap=6):
                items = list(items)
                active = []
                since = gap
                while items or active:
                    if items and len(active) < nslots and since >= gap:
                        used = {s_ for s_, _ in active}
                        slot = [s_ for s_ in range(nslots) if s_ not in used][0]
                        active.append((slot, make_gen(items.pop(0), slot)))
                        since = 0
                    for ent in list(active):
                        try:
                            next(ent[1])
                        except StopIteration:
                            active.remove(ent)
                    since += 1

            def u_mm(si, bank):
                for h in range(4):
                    OP("pe", lambda e: e.matmul(pb(bank, h * 128, (h + 1) * 128), lhsT=kdS[si][:, h * 128:(h + 1) * 128],
                                                rhs=vS[si][:, h * 128:(h + 1) * 128], start=True, stop=True), r=[kdS[si], vS[si]], w=[P[bank]])

            def state_update(si, bank, lo, hi, store=None):
                for h in range(4):
                    if store is not None:
                        OP("act", lambda e: e.activation(out=store[lo:hi, h, :], in_=S[lo:hi, h, :], func=AF.Copy, scale=decS[si][lo:hi, h:h + 1]),
                           r=[S, decS[si]], w=[store])
                    OP("dve", lambda e: e.scalar_tensor_tensor(out=S[lo:hi, h, :], in0=S[lo:hi, h, :], scalar=decS[si][lo:hi, h:h + 1],
                                                               in1=pb(bank, h * 128, (h + 1) * 128)[lo:hi, :], op0=ALU.mult, op1=ALU.add),
                       r=[S, decS[si], P[bank]], w=[S])

            for b in range(2):
              with ExitStack() as sp_:
                winb = sb(sp_, "winb", [128, 8, 2080], BF16)
                fw.dma("sp", lambda q: q.dma_start(out=winb[:], in_=wi16_d.rearrange("(k p) n -> p k n", p=128)), r=[WI16], w=[winb])
                W = []
                for s_ in range(2):
                    W.append(dict(
                        xmT=sb(sp_, f"xmT{s_}", [128, 8, 128], BF16),
                        qk=sb(sp_, f"qk{s_}", [128, 512], F32), rsb=sb(sp_, f"rsb{s_}", [128, 32], F32), rT=sb(sp_, f"rT{s_}", [32, 128], F32),
                        Lt=sb(sp_, f"Lt{s_}", [128, 512], F32), Em=sb(sp_, f"Em{s_}", [128, 512], F32), qt=sb(sp_, f"qt{s_}", [128, 512], BF16),
                        junk=sb(sp_, f"junk{s_}", [128, D], BF16), tv=sb(sp_, f"tvp{s_}", [128, 8], F32)))

                xring = [sb(sp_, f"xring{i_}", [128, D], F32) for i_ in range(3)]
                xsrc = {}
                xissued = set()

                def x_issue(g):
                    if g in xissued or g not in xsrc:
                        return
                    xissued.add(g)
                    fw.dma("sp", lambda q: q.dma_start(out=xring[g % 3][:], in_=xsrc[g]), w=[xring[g % 3]])

                def prep_gen(item, slot):
                    g, src, is_ctx, si, a1, b1 = item
                    w_ = W[slot]
                    b0, b1k, b2, b3 = SLOT_BANKS[slot]
                    xmT, qk, rsb, rT, Lt, Em, qt, junk, tv = (w_[k] for k in ("xmT", "qk", "rsb", "rT", "Lt", "Em", "qt", "junk", "tv"))
                    Ep, gsil = Lt, Em
                    xi = xring[g % 3]
                    x_issue(g)
                    x_issue(g + 1)
                    OP("act", lambda e: e.activation(out=junk[:], in_=xi[:], func=AF.Square, accum_out=tv[:, 0:1]), r=[xi], w=[junk, tv])
                    rstd_ss(tv, tv[:, 0:1], tv[:, 1:2], D)
                    OP("act", lambda e: e.activation(out=xi[:], in_=xi[:], func=AF.Copy, scale=tv[:, 1:2]), r=[xi, tv], w=[xi])
                    yield
                    conv_step(pace=[xi])
                    for hf in range(2):
                        for c in range(4):
                            k = hf * 4 + c
                            OP("pe", lambda e: e.transpose(out=pb(b0, c * 128, (c + 1) * 128), in_=xi[:, k * 128:(k + 1) * 128], identity=ident),
                               r=[xi, cf], w=[P[b0]])
                        for c in range(4):
                            k = hf * 4 + c
                            OP("act", lambda e: e.activation(out=xmT[:, k, :], in_=pb(b0, c * 128, (c + 1) * 128), func=AF.Identity,
                                                             scale=a1[:, k:k + 1], bias=b1[:, k:k + 1]), r=[P[b0], a1fm, b1fm], w=[xmT])
                        yield
                    groups = [(0, 512, "qk"), (512, 1024, "v")] + ([] if is_ctx else [(1024, 1536, "g"), (1536, 2048, "pool")]) + [(2048, 2080, "r")]
                    for gi, (lo, hi, nm) in enumerate(groups):
                        bk = b1k
                        for k in range(8):
                            OP("pe", lambda e: e.matmul(pb(bk, 0, hi - lo), lhsT=xmT[:, k, :], rhs=winb[:, k, lo:hi], start=(k == 0), stop=(k == 7)),
                               r=[xmT, winb], w=[P[bk]])
                        if nm == "qk":
                            OP("dve", lambda e: e.tensor_copy(out=qk[:], in_=pb(bk)), r=[P[bk]], w=[qk])
                        elif nm == "v":
                            OP("act", lambda e: e.activation(out=vS[si][:], in_=pb(bk), func=AF.Copy), r=[P[bk]], w=[vS[si]])
                        elif nm == "g":
                            OP("act", lambda e: e.activation(out=gsil[:], in_=pb(bk), func=AF.Silu), r=[P[bk]], w=[gsil])
                            OP("dve", lambda e: e.tensor_tensor(out=gwS[si][:], in0=gsil[:], in1=gnwb[:], op=ALU.mult), r=[gsil, gnwb], w=[gwS[si]])
                        elif nm == "pool":
                            OP("dve", lambda e: e.tensor_copy(out=plS[si][:], in_=pb(bk)), r=[P[bk]], w=[plS[si]])
                        else:
                            OP("dve", lambda e: e.tensor_copy(out=rsb[:], in_=pb(bk, 0, 32)), r=[P[bk]], w=[rsb])
                        yield
                    OP("pe", lambda e: e.transpose(out=pb(b2, 0, 128)[0:32, :], in_=rsb[:, 0:32], identity=ident), r=[rsb, cf], w=[P[b2]])
                    OP("dve", lambda e: e.tensor_copy(out=rT[:], in_=pb(b2, 0, 128)[0:32, :]), r=[P[b2]], w=[rT])
                    OP("pe", lambda e: e.matmul(pb(b2), lhsT=rT[:], rhs=wgk[:], start=True, stop=False), r=[rT, wgk], w=[P[b2]])
                    OP("pe", lambda e: e.matmul(pb(b2), lhsT=ones1, rhs=bgk[:], start=False, stop=True), r=[cf, bgk], w=[P[b2]])
                    OP("act", lambda e: e.activation(out=Lt[:], in_=pb(b2), func=AF.Exp, scale=-1.0), r=[P[b2]], w=[Lt])
                    OP("act", lambda e: e.activation(out=Lt[:], in_=Lt[:], func=AF.Ln, bias=1.0), r=[Lt], w=[Lt])
                    yield
                    for h in range(4):
                        for d in range(2):
                            m1 = cf[:, (CF_M1F if d == 0 else CF_M1B):(CF_M1F if d == 0 else CF_M1B) + 128]
                            c0 = h * 128 + d * 64
                            OP("pe", lambda e: e.matmul(pb(b2, c0, c0 + 64), lhsT=m1, rhs=Lt[:, c0:c0 + 64], start=True, stop=True),
                               r=[cf, Lt], w=[P[b2]])
                    for h in range(4):
                        OP("pe", lambda e: e.matmul(pb(b3, h, h + 1), lhsT=Lt[:, h * 128:(h + 1) * 128], rhs=cf[:, CF_NEG:CF_NEG + 1],
                                                    start=True, stop=True), r=[Lt, cf], w=[P[b3]])
                    OP("act", lambda e: e.activation(out=decS[si][:], in_=pb(b3, 0, 4), func=AF.Exp), r=[P[b3]], w=[decS[si]])
                    OP("act", lambda e: e.activation(out=Em[:], in_=pb(b2), func=AF.Exp, scale=-1.0), r=[P[b2]], w=[Em])
                    k4 = qk[:, 256:512].rearrange("p (h k) -> p h k", h=4)
                    for d in range(2):
                        OP("dve", lambda e: e.tensor_tensor(
                            out=kdS[si][:].rearrange("p (h d k) -> p h d k", h=4, d=2)[:, :, d, :], in0=k4,
                            in1=Em[:].rearrange("p (h d k) -> p h d k", h=4, d=2)[:, :, d, :], op=ALU.mult), r=[qk, Em], w=[kdS[si]])
                    yield
                    if is_ctx:
                        u_mm(si, b3)
                        return
                    OP("act", lambda e: e.activation(out=Ep[:], in_=pb(b2), func=AF.Exp), r=[P[b2]], w=[Ep])
                    q4 = qk[:, 0:256].rearrange("p (h k) -> p h k", h=4)
                    for d in range(2):
                        OP("dve", lambda e: e.scalar_tensor_tensor(
                            out=qt[:].rearrange("p (h d k) -> p h d k", h=4, d=2)[:, :, d, :], in0=q4, scalar=0.125,
                            in1=Ep[:].rearrange("p (h d k) -> p h d k", h=4, d=2)[:, :, d, :], op0=ALU.mult, op1=ALU.mult), r=[qk, Ep], w=[qt])
                    yield
                    for srcT, dstT, bk, eng in ((qt, qTS[si], b0, "act"), (kdS[si], kTS[si], b1k, "dve")):
                        for h in range(4):
                            OP("pe", lambda e: e.transpose(out=pbb(bk)[:, h * 128:(h + 1) * 128], in_=srcT[:, h * 128:(h + 1) * 128], identity=identb),
                               r=[srcT, cb], w=[P[bk]])
                        if eng == "act":
                            OP("act", lambda e: e.activation(out=dstT[:].rearrange("p h t -> p (h t)"), in_=pbb(bk)[:, 0:512], func=AF.Copy), r=[P[bk]], w=[dstT])
                        else:
                            OP("dve", lambda e: e.tensor_copy(out=dstT[:].rearrange("p h t -> p (h t)"), in_=pbb(bk)[:, 0:512]), r=[P[bk]], w=[dstT])
                    yield
                    u_mm(si, b3)
                    state_update(si, b3, 0, 64, store=dSS[si])

                OP("pool", lambda e: e.memset(S[:], 0.0), w=[S])
                for j in range(2):
                    xsrc[j] = ctx_d[b * 256 + j * 128: b * 256 + (j + 1) * 128, :]
                for i in range(16):
                    xsrc[2 + i] = x_d[b * 2048 + i * 128: b * 2048 + (i + 1) * 128, :]
                run_pipelined(prep_gen, [(j, xsrc[j], True, 16 + j, a1fm[:, 2, :], b1fm[:, 2, :]) for j in range(2)], gap=0)
                for j in (0, 1):
                    state_update(16 + j, SLOT_BANKS[j][3], 0, 64)
                for j in (1, 0):
                    state_update(16 + j, SLOT_BANKS[j][3], 64, 128)
                if stop == "ctx":
                    DUMP(S, S[:].rearrange("p h v -> p (h v)"), 0, 512)
                CHK("ctx")
                run_pipelined(prep_gen, [(2 + i, xsrc[2 + i], False, i, a1fm[:, b, :], b1fm[:, b, :]) for i in range(16)])
                fw.barrier()
              CHK("prep")
              with ExitStack() as sk_:
                woutb = sb(sk_, "woutb", [128, 8, D], BF16)
                g1b = sb(sk_, "g1b", [128, D], F32); a2b = sb(sk_, "a2b", [128, D], F32); b2b = sb(sk_, "b2b", [128, D], F32)
                fw.dma("sp", lambda q: q.dma_start(out=woutb[:], in_=wo16_d.rearrange("(k p) n -> p k n", p=128)), r=[WO16], w=[woutb])
                fw.dma("sp", lambda q: q.dma_start(out=g1b[:], in_=mod_d[b:b + 1, 2 * D:3 * D].partition_broadcast(128)), r=[MODD], w=[g1b])
                fw.dma("sp", lambda q: q.dma_start(out=b2b[:], in_=nmlp_d), w=[b2b])
                fw.dma("sp", lambda q: q.dma_start(out=a2b[:], in_=mod_d[b:b + 1, 4 * D:5 * D].partition_broadcast(128)), r=[MODD], w=[a2b])
                OP("dve", lambda e: e.scalar_tensor_tensor(out=a2b[:], in0=a2b[:], scalar=1.0, in1=b2b[:], op0=ALU.add, op1=ALU.mult),
                   r=[a2b, b2b], w=[a2b])
                fw.dma("sp", lambda q: q.dma_start(out=b2b[:], in_=mod_d[b:b + 1, 3 * D:4 * D].partition_broadcast(128)), r=[MODD], w=[b2b])
                if b == 0:
                    zt = sb(sk_, "zt", [128, D], F32)
                    OP("pool", lambda e: e.memset(zt[:], 0.0), w=[zt])
                    for i_ in range(NTILE + 1):
                        zero_todo.append(lambda i_=i_: fw.dma("sp", lambda q: q.dma_start(out=yacc_d[i_ * 128:(i_ + 1) * 128, :], in_=zt[:]), r=[zt]))
                    zero_todo.append(lambda: fw.dma("sp", lambda q: q.dma_start(out=x2_d[NT:NT + 128, :], in_=zt[:, 0:512].bitcast(BF16)), r=[zt]))
                V = []
                for s_ in range(2):
                    V.append(dict(
                        xr=sb(sk_, f"xr{s_}", [128, D], F32), h1=sb(sk_, f"h1{s_}", [128, D], F32), mix=sb(sk_, f"mix{s_}", [128, D], BF16),
                        mixT=sb(sk_, f"mixT{s_}", [128, 8, 128], BF16), rawT=sb(sk_, f"rawT{s_}", [128, 512], BF16),
                        junk=sb(sk_, f"junkb{s_}", [128, D], BF16), aTf=sb(sk_, f"aTf{s_}", [128, 128], BF16), aTb=sb(sk_, f"aTb{s_}", [128, 128], BF16),
                        tv=sb(sk_, f"tvb{s_}", [128, 32], F32), lg=sb(sk_, f"lg{s_}", [128, E], F32), msk=sb(sk_, f"msk{s_}", [128, E], F32),
                        mskb=sb(sk_, f"mskb{s_}", [128, E], BF16), t8=sb(sk_, f"t8{s_}", [128, 8], F32), addr=sb(sk_, f"addr{s_}", [128, E], F32),
                        oh=sb(sk_, f"oh{s_}", [128, E], F32), dst=sb(sk_, f"dst{s_}", [128, 8], F32),
                        dsi=[sb(sk_, f"dsi{s_}{r_}", [128, 4], I32) for r_ in range(4)],
                        pk=[sb(sk_, f"pk{s_}{r_}", [128, 4, 2], I32) for r_ in range(4)], e4=sb(sk_, f"e4{s_}", [128, 4], F32)))

                def back_gen(i, slot):
                    v_ = V[slot]
                    t0, t1, t2, t3 = SLOT_BANKS[slot]
                    xr, h1, mix, mixT, rawT, junk, aTf, aTb, tv = (v_[k] for k in ("xr", "h1", "mix", "mixT", "rawT", "junk", "aTf", "aTb", "tv"))
                    lg, msk, mskb, t8, addr, oh, dst, dsi, pki, e4 = (v_[k] for k in ("lg", "msk", "mskb", "t8", "addr", "oh", "dst", "dsi", "pk", "e4"))
                    dsi, pki = dsi[(i // 2) % 4], pki[(i // 2) % 4]
                    x2, x2T = mix, mixT
                    tg = b * 16 + i
                    rows = slice(tg * 128, (tg + 1) * 128)
                    u_mm(i, t0)
                    state_update(i, t0, 64, 128, store=dSS[i])
                    fw.dma("sp", lambda q: q.dma_start(out=xr[:], in_=x_d[rows, :]), w=[xr])
                    yield
                    conv_step(pace=[xr])
                    for _ in range(3):
                        if zero_todo:
                            zero_todo.pop(0)()
                    for h in range(4):
                        OP("pe", lambda e: e.matmul(pb(t1, 0, 128), lhsT=kTS[i][0:64, h, :], rhs=qTS[i][0:64, h, :], start=True, stop=True),
                           r=[kTS[i], qTS[i]], w=[P[t1]])
                        OP("pe", lambda e: e.matmul(pb(t2, 0, 128), lhsT=kTS[i][64:128, h, :], rhs=qTS[i][64:128, h, :], start=True, stop=True),
                           r=[kTS[i], qTS[i]], w=[P[t2]])
                        OP("dve", lambda e: e.tensor_tensor(out=aTf[:], in0=pb(t1, 0, 128), in1=cb[:, CB_MF:CB_MF + 128], op=ALU.mult), r=[P[t1], cb], w=[aTf])
                        OP("dve", lambda e: e.tensor_tensor(out=aTb[:], in0=pb(t2, 0, 128), in1=cb[:, CB_MB:CB_MB + 128], op=ALU.mult), r=[P[t2], cb], w=[aTb])
                        o_ps = pb(t3, h * 128, (h + 1) * 128)
                        vh = vS[i][:, h * 128:(h + 1) * 128]
                        OP("pe", lambda e: e.matmul(o_ps, lhsT=aTf[:], rhs=vh, start=True, stop=False), r=[aTf, vS[i]], w=[P[t3]])
                        OP("pe", lambda e: e.matmul(o_ps, lhsT=aTb[:], rhs=vh, start=False, stop=False), r=[aTb, vS[i]], w=[P[t3]])
                        OP("pe", lambda e: e.matmul(o_ps, lhsT=qTS[i][:, h, :], rhs=dSS[i][:, h, :], start=False, stop=True), r=[qTS[i], dSS[i]], w=[P[t3]])
                        yield
                    for h in range(4):
                        OP("act", lambda e: e.activation(out=junk[:, h * 128:(h + 1) * 128], in_=pb(t3, h * 128, (h + 1) * 128), func=AF.Square,
                                                         accum_out=tv[:, 8 + h:9 + h]), r=[P[t3]], w=[junk, tv])
                    rstd_ss(tv, tv[:, 8:12], tv[:, 12:16], 128)
                    for h in range(4):
                        OP("dve", lambda e: e.scalar_tensor_tensor(out=mix[:, h * 128:(h + 1) * 128], in0=pb(t3, h * 128, (h + 1) * 128),
                                                                   scalar=tv[:, 12 + h:13 + h], in1=gwS[i][:, h * 128:(h + 1) * 128],
                                                                   op0=ALU.mult, op1=ALU.mult), r=[P[t3], tv, gwS[i]], w=[mix])
                    yield
                    for g in range(4):
                        js = [j for j in range(16) if (g, i, j) in _POOL_INDEX]
                        for n, j in enumerate(js):
                            m = _POOL_INDEX[(g, i, j)]
                            OP("pe", lambda e: e.matmul(pb(t0, g * 128, (g + 1) * 128), lhsT=plS[j][:, g * 128:(g + 1) * 128],
                                                        rhs=cb[:, CB_POOL + m * 128:CB_POOL + (m + 1) * 128], start=(n == 0), stop=(n == len(js) - 1)),
                               r=[plS[j], cb], w=[P[t0]])
                    OP("act", lambda e: e.activation(out=rawT[:], in_=pb(t0), func=AF.Copy), r=[P[t0]], w=[rawT])
                    yield
                    for g in range(4):
                        OP("pe", lambda e: e.matmul(pb(t1, g * 128, (g + 1) * 128), lhsT=rawT[:, g * 128:(g + 1) * 128], rhs=wpoolb[:, g, :],
                                                    start=True, stop=True), r=[rawT, wpoolb], w=[P[t1]])
                    for g in range(4):
                        OP("dve", lambda e: e.scalar_tensor_tensor(out=mix[:, 512 + g * 128:512 + (g + 1) * 128], in0=pb(t1, g * 128, (g + 1) * 128),
                                                                   scalar=cf[:, CF_INV + i * 4 + g:CF_INV + i * 4 + g + 1],
                                                                   in1=pscb[:, g * 128:(g + 1) * 128], op0=ALU.mult, op1=ALU.mult),
                           r=[P[t1], cf, pscb], w=[mix])
                    yield
                    for k in range(8):
                        OP("pe", lambda e: e.transpose(out=pbb(t2)[:, k * 128:(k + 1) * 128], in_=mix[:, k * 128:(k + 1) * 128], identity=identb),
                           r=[mix, cb], w=[P[t2]])
                    OP("act", lambda e: e.activation(out=mixT[:].rearrange("p k t -> p (k t)"), in_=pbb(t2), func=AF.Copy), r=[P[t2]], w=[mixT])
                    yield
                    for n, bk in enumerate((t0, t1)):
                        for k in range(8):
                            OP("pe", lambda e: e.matmul(pb(bk), lhsT=mixT[:, k, :], rhs=woutb[:, k, n * 512:(n + 1) * 512], start=(k == 0), stop=(k == 7)),
                               r=[mixT, woutb], w=[P[bk]])
                        OP("dve", lambda e: e.tensor_tensor(out=h1[:, n * 512:(n + 1) * 512], in0=pb(bk), in1=g1b[:, n * 512:(n + 1) * 512], op=ALU.mult),
                           r=[P[bk], g1b], w=[h1])
                    OP("dve", lambda e: e.tensor_tensor(out=h1[:], in0=h1[:], in1=xr[:], op=ALU.add), r=[h1, xr], w=[h1])
                    fw.dma("sp", lambda q: q.dma_start(out=h1_d[rows, :], in_=h1[:]), r=[h1], w=[H1D])
                    yield
                    OP("act", lambda e: e.activation(out=junk[:], in_=h1[:], func=AF.Square, accum_out=tv[:, 16:17]), r=[h1], w=[junk, tv])
                    rstd_ss(tv, tv[:, 16:17], tv[:, 17:18], D)
                    OP("dve", lambda e: e.scalar_tensor_tensor(out=xr[:], in0=h1[:], scalar=tv[:, 17:18], in1=a2b[:], op0=ALU.mult, op1=ALU.mult),
                       r=[h1, tv, a2b], w=[xr])
                    OP("dve", lambda e: e.tensor_tensor(out=x2[:], in0=xr[:], in1=b2b[:], op=ALU.add), r=[xr, b2b], w=[x2])
                    fw.dma("sp", lambda q: q.dma_start(out=x2_d[rows, :], in_=x2[:]), r=[x2], w=[X2D])
                    yield
                    for k in range(8):
                        OP("pe", lambda e: e.transpose(out=pbb(t2)[:, k * 128:(k + 1) * 128], in_=x2[:, k * 128:(k + 1) * 128], identity=identb),
                           r=[x2, cb], w=[P[t2]])
                    OP("act", lambda e: e.activation(out=x2T[:].rearrange("p k t -> p (k t)"), in_=pbb(t2), func=AF.Copy), r=[P[t2]], w=[x2T])
                    for k in range(8):
                        OP("pe", lambda e: e.matmul(pb(t3, 0, E), lhsT=x2T[:, k, :], rhs=wrb[:, k, :], start=(k == 0), stop=(k == 7)), r=[x2T, wrb], w=[P[t3]])
                    OP("dve", lambda e: e.tensor_tensor(out=lg[:], in0=pb(t3, 0, E), in1=brb[:], op=ALU.add), r=[P[t3], brb], w=[lg])
                    OP("dve", lambda e: e.max(out=t8[:], in_=lg[:]), r=[lg], w=[t8])
                    OP("dve", lambda e: e.tensor_scalar(out=msk[:], in0=lg[:], scalar1=t8[:, 3:4], scalar2=None, op0=ALU.is_ge), r=[lg, t8], w=[msk])
                    OP("dve", lambda e: e.tensor_copy(out=mskb[:], in_=msk[:]), r=[msk], w=[mskb])
                    yield
                    OP("dve", lambda e: e.tensor_scalar(out=tv[:, 20:21], in0=t8[:, 0:1], scalar1=-1.0, scalar2=None, op0=ALU.mult), r=[t8, tv], w=[tv])
                    OP("act", lambda e: e.activation(out=e4[:], in_=t8[:, 0:4], func=AF.Exp, bias=tv[:, 20:21], accum_out=tv[:, 21:22]),
                       r=[t8, tv], w=[e4, tv])
                    OP("dve", lambda e: e.reciprocal(out=tv[:, 22:23], in_=tv[:, 21:22]), r=[tv], w=[tv])
                    OP("dve", lambda e: e.tensor_scalar(out=e4[:], in0=e4[:], scalar1=tv[:, 22:23], scalar2=None, op0=ALU.mult), r=[e4, tv], w=[e4])
                    OP("pe", lambda e: e.matmul(pb(t3, 64, 64 + E), lhsT=cb[:, CB_SU:CB_SU + 128], rhs=mskb[:], start=True, stop=True), r=[cb, mskb], w=[P[t3]])
                    OP("pe", lambda e: e.matmul(pb(t3, 128, 128 + E), lhsT=cb[:, CB_ONES:CB_ONES + 128], rhs=mskb[:], start=True, stop=True), r=[cb, mskb], w=[P[t3]])
                    OP("dve", lambda e: e.tensor_tensor(out=addr[:], in0=pb(t3, 64, 64 + E), in1=carry[:], op=ALU.add), r=[P[t3], carry], w=[addr])
                    OP("dve", lambda e: e.tensor_tensor(out=carry[:], in0=pb(t3, 128, 128 + E), in1=carry[:], op=ALU.add), r=[P[t3], carry], w=[carry])
                    OP("dve", lambda e: e.tensor_tensor(out=addr[:], in0=addr[:], in1=cf[:, CF_ECAP:CF_ECAP + E], op=ALU.add), r=[addr, cf], w=[addr])
                    for k in range(4):
                        OP("dve", lambda e: e.tensor_scalar(out=oh[:], in0=lg[:], scalar1=t8[:, k:k + 1], scalar2=None, op0=ALU.is_equal), r=[lg, t8], w=[oh])
                        OP("dve", lambda e: e.tensor_tensor(out=oh[:], in0=oh[:], in1=addr[:], op=ALU.mult), r=[oh, addr], w=[oh])
                        OP("dve", lambda e: e.reduce_sum(out=dst[:, k:k + 1], in_=oh[:], axis=mybir.AxisListType.X), r=[oh], w=[dst])
                    OP("dve", lambda e: e.tensor_copy(out=dsi[:], in_=dst[:, 0:4]), r=[dst], w=[dsi])
                    OP("dve", lambda e: e.tensor_scalar(out=pki[:, :, 0], in0=z4[:], scalar1=cf[:, CF_TOK:CF_TOK + 1], scalar2=float(tg * 128),
                                                        op0=ALU.add, op1=ALU.add), r=[z4, cf], w=[pki])
                    OP("dve", lambda e: e.tensor_copy(out=pki[:, :, 1], in_=e4[:].bitcast(I32)), r=[e4], w=[pki])
                    for k in range(4):
                        fw.dma("pool", lambda q: q.indirect_dma_start(out=lists_d, out_offset=bass.IndirectOffsetOnAxis(ap=dsi[:, k:k + 1], axis=0),
                                                                      in_=pki[:, k, :], in_offset=None), r=[pki, dsi])

                run_pipelined(back_gen, list(range(15, -1, -1)))
                while zero_todo:
                    zero_todo.pop(0)()
                fw.barrier()
            nbf = sb(st, "nbf", [128, 2 * E], F32)
            OP("pool", lambda e: e.memset(nbf[:], 0.0), w=[nbf])
            for j in range(NBMAX):
                OP("dve", lambda e: e.scalar_tensor_tensor(out=nbf[:, 0:E], in0=carry[:], scalar=float(128 * j), in1=nbf[:, 0:E],
                                                           op0=ALU.is_gt, op1=ALU.add), r=[carry, nbf], w=[nbf])
            OP("dve", lambda e: e.tensor_copy(out=nbi[:], in_=nbf[:, 0:E]), r=[nbf], w=[nbi])
            fw.dma("sp", lambda q: q.dma_start(out=nblk_d, in_=nbi[0:1, :]), r=[nbi], w=[NBLKD])
            fw.barrier()

        if phases >= 3:
          with ExitStack() as st:
            wgu = [sb(st, f"wgu{i}", [128, 8, 2 * D], BF16) for i in range(2)]
            wdn = [sb(st, f"wdn{i}", [128, 8, D], BF16) for i in range(2)]
            bst = sb(st, "bst", [1, 3 * D], F32)
            ids = [sb(st, f"ids{i}", [128, 2], I32) for i in range(3)]
            xg = [sb(st, f"xg{i}", [128, D], BF16) for i in range(3)]
            xgT = [sb(st, f"xgT{i}", [128, 8, 128], BF16) for i in range(2)]
            gc = [[sb(st, f"gc{p}{h}", [128, 512], F32) for h in range(2)] for p in range(1)]
            sg = [[sb(st, f"sg{p}{h}", [128, 512], F32) for h in range(2)] for p in range(1)]
            uc = [[sb(st, f"uc{p}{h}", [128, 512], F32) for h in range(2)] for p in range(1)]
            actb = [[sb(st, f"actb{p}{h}", [128, 512], BF16) for h in range(2)] for p in range(1)]
            actT = [[sb(st, f"actT{p}{h}", [128, 4, 128], BF16) for h in range(2)] for p in range(1)]
            yw = [sb(st, f"yw{i}", [128, D], F32) for i in range(2)]
            ones1b = cb[0:1, CB_ONES:CB_ONES + 128]
            bhl = [[sb(st, f"bhl{i}{j}", [1, 3 * D], BF16) for j in range(1)] for i in range(2)]
            regs = nc.alloc_registers("nblk", [ET.PE, ET.Activation, ET.DVE, ET.Pool, ET.SP])
            PGh = [T(None), T(None)]
            PD = T(None); PT0 = P[0]
            PT1h = [T(None), T(None)]

            def load_bias(ex):
                fw.dma("sp", lambda q: q.dma_start(out=bst[0:1, 0:2 * D], in_=bgu_d[ex:ex + 1, :]), w=[bst])
                fw.dma("sp", lambda q: q.dma_start(out=bst[0:1, 2 * D:3 * D], in_=bdn_d[ex:ex + 1, :]), w=[bst])
                hi = bhl[ex % 2][0]
                OP("dve", lambda e: e.tensor_copy(out=hi[:], in_=bst[:]), r=[bst], w=[hi])

            wgh = [[T(None) for _ in range(8)] for _ in range(2)]
            wdh = [[T(None) for _ in range(8)] for _ in range(2)]

            def weight_pieces(ex):
                wg, wd = wgu[ex % 2], wdn[ex % 2]
                pcs = []
                for k in range(8):
                    pcs.append(lambda k=k: fw.dma("sp", lambda q: q.dma_start(out=wg[:, k, :], in_=wgu16_d[ex, k * 128:(k + 1) * 128, :]),
                                                  r=[WCV[ex][0]], w=[wgh[ex % 2][k]]))
                for k in range(8):
                    pcs.append(lambda k=k: fw.dma("sp", lambda q: q.dma_start(out=wd[:, k, :], in_=wdn16_d[ex, k * 128:(k + 1) * 128, :]),
                                                  r=[WCV[ex][1]], w=[wdh[ex % 2][k]]))
                return pcs

            ids0 = [sb(st, f"ids0{i}", [128, 2], I32) for i in range(2)]
            xg0 = [sb(st, f"xg0{i}", [128, D], BF16) for i in range(2)]

            def blk_bufs(ex, j):
                nbidx = ex * nbmax + j
                return (ids0[ex % 2], xg0[ex % 2]) if j == 0 else (ids[nbidx % 3], xg[nbidx % 3])

            def issue_fetch(ex, j):
                idt, xgt = blk_bufs(ex, j)
                blk = ex * NBMAX + j
                fw.dma("pool", lambda q: q.dma_start(out=idt[:], in_=lists_d[blk * 128:(blk + 1) * 128, :]), r=[LISTS], w=[idt])
                fw.dma("pool", lambda q: q.indirect_dma_start(out=xgt[:], out_offset=None, in_=x2_d,
                                                              in_offset=bass.IndirectOffsetOnAxis(ap=idt[:, 0:1], axis=0)), r=[X2D, idt], w=[xgt])

            def emit_tx(ex, j):
                nbidx = ex * nbmax + j
                xgt, xT = blk_bufs(ex, j)[1], xgT[nbidx % 2]
                for k in range(8):
                    OP("pe", lambda e: e.transpose(out=pbb(0)[:, k * 128:(k + 1) * 128], in_=xgt[:, k * 128:(k + 1) * 128], identity=identb),
                       r=[xgt, cb], w=[PT0])
                OP("act", lambda e: e.activation(out=xT[:].rearrange("p k t -> p (k t)"), in_=pbb(0), func=AF.Copy), r=[PT0], w=[xT])

            while conv_todo:
                conv_step()
            load_bias(0)
            issue_fetch(0, 0)
            for pc in weight_pieces(0):
                pc()
            for ex in range(E):
                pcs = []
                if ex + 1 < E:
                    issue_fetch(ex + 1, 0)
                    load_bias(ex + 1)
                    pcs = weight_pieces(ex + 1)
                wg, wd = wgu[ex % 2], wdn[ex % 2]
                bhi = bhl[ex % 2][0]
                for e_ in fw.ENG:
                    fw.sync_to(e_, [nbi])
                for reg in regs:
                    nc.reg_load(reg, nbi[0:1, ex:ex + 1])

                def emit_block(j):
                    fw.begin_if_cmp(regs, j, "IS_GT")
                    nbidx = ex * nbmax + j
                    (idt, xgt), ywt = blk_bufs(ex, j), yw[nbidx % 2]
                    par = 0
                    xT = xgT[nbidx % 2]
                    if j + 1 < nbmax:
                        issue_fetch(ex, j + 1)
                    emit_tx(ex, j)
                    for hf in range(2):
                        for gi, c0 in enumerate((hf * 512, D + hf * 512)):
                            bk = 2 + 2 * hf + gi
                            for k in range(8):
                                OP("pe", lambda e: e.matmul(pb(bk), lhsT=xT[:, k, :], rhs=wg[:, k, c0:c0 + 512], start=(k == 0), stop=False),
                                   r=[xT, wgh[ex % 2][k]], w=[PGh[hf]])
                            OP("pe", lambda e: e.matmul(pb(bk), lhsT=ones1b, rhs=bhi[0:1, c0:c0 + 512], start=False, stop=True), r=[cb, bhi], w=[PGh[hf]])
                    for hf in range(2):
                        g_, s_, u_, a_, aT_ = gc[par][hf], sg[par][hf], uc[par][hf], actb[par][hf], actT[par][hf]
                        OP("dve", lambda e: e.tensor_scalar(out=g_[:], in0=pb(2 + 2 * hf), scalar1=7.0, scalar2=None, op0=ALU.min), r=[PGh[hf]], w=[g_])
                        OP("act", lambda e: e.activation(out=s_[:], in_=g_[:], func=AF.Sigmoid, scale=1.702), r=[g_], w=[s_])
                        OP("dve", lambda e: e.tensor_scalar(out=u_[:], in0=pb(3 + 2 * hf), scalar1=-7.0, scalar2=7.0, op0=ALU.max, op1=ALU.min), r=[PGh[hf]], w=[u_])
                        OP("dve", lambda e: e.scalar_tensor_tensor(out=u_[:], in0=u_[:], scalar=1.0, in1=g_[:], op0=ALU.add, op1=ALU.mult), r=[u_, g_], w=[u_])
                        OP("dve", lambda e: e.tensor_tensor(out=a_[:], in0=u_[:], in1=s_[:], op=ALU.mult), r=[u_, s_], w=[a_])
                        for c in range(4):
                            OP("pe", lambda e: e.transpose(out=pbb(1)[:, (hf * 4 + c) * 128:(hf * 4 + c + 1) * 128], in_=a_[:, c * 128:(c + 1) * 128], identity=identb),
                               r=[a_, cb], w=[PT1h[hf]])
                        OP("act", lambda e: e.activation(out=aT_[:].rearrange("p k t -> p (k t)"), in_=pbb(1)[:, hf * 512:(hf + 1) * 512], func=AF.Copy),
                           r=[PT1h[hf]], w=[aT_])
                        for n in range(2):
                            for k in range(4 * hf, 4 * hf + 4):
                                OP("pe", lambda e: e.matmul(pb(6 + n), lhsT=aT_[:, k % 4, :], rhs=wd[:, k, n * 512:(n + 1) * 512], start=(k == 0), stop=False),
                                   r=[aT_, wdh[ex % 2][k]], w=[PD])
                    for n in range(2):
                        OP("pe", lambda e: e.matmul(pb(6 + n), lhsT=ones1b, rhs=bhi[0:1, 2 * D + n * 512:2 * D + (n + 1) * 512], start=False, stop=True), r=[cb, bhi], w=[PD])
                    OP("act", lambda e: e.activation(out=ywt[:], in_=psum[:, 6 * 512:8 * 512], func=AF.Copy, scale=idt[:, 1:2].bitcast(F32)), r=[PD, idt], w=[ywt])
                    fw.dma("pool", lambda q: q.indirect_dma_start(out=yacc_d, out_offset=bass.IndirectOffsetOnAxis(ap=idt[:, 0:1], axis=0),
                                                                  in_=ywt[:], in_offset=None, compute_op=ALU.add), r=[ywt, idt, YACC], w=[YACC])

                while pcs:
                    pcs.pop(0)()
                for j in range(nbmax):
                    emit_block(j)
                for j in range(nbmax):
                    fw.end_if()
            fw.barrier()

        if phases >= 4:
          with ExitStack() as st:
            g2b = [sb(st, f"g2b{i}", [128, D], F32) for i in range(2)]
            fnwb = sb(st, "fnwb", [128, D], F32)
            NF = 4
            F_ = [dict(h=sb(st, f"fh{i}", [128, D], F32), y=sb(st, f"fy{i}", [128, D], F32), o=sb(st, f"fo{i}", [128, D], F32),
                       junk=sb(st, f"fj{i}", [128, D], BF16), tv=sb(st, f"ftv{i}", [128, 4], F32), out=T(None)) for i in range(NF)]
            fw.dma("sp", lambda q: q.dma_start(out=fnwb[:], in_=fnw_d), w=[fnwb])
            for b in range(2):
                fw.dma("sp", lambda q: q.dma_start(out=g2b[b][:], in_=mod_d[b:b + 1, 5 * D:6 * D].partition_broadcast(128)), r=[MODD], w=[g2b[b]])

            def final_gen(tg, slot):
                f_ = F_[slot]
                h_, y_, o_, jk, tv, OUT = f_["h"], f_["y"], f_["o"], f_["junk"], f_["tv"], f_["out"]
                rows = slice(tg * 128, (tg + 1) * 128)
                fw.dma("sp", lambda q: q.dma_start(out=h_[:], in_=h1_d[rows, :]), r=[H1D], w=[h_])
                fw.dma("sp", lambda q: q.dma_start(out=y_[:], in_=yacc_d[rows, :]), r=[YACC], w=[y_])
                yield
                OP("dve", lambda e: e.tensor_tensor(out=y_[:], in0=y_[:], in1=g2b[tg // 16][:], op=ALU.mult), r=[y_, g2b[tg // 16]], w=[y_])
                OP("dve", lambda e: e.tensor_tensor(out=h_[:], in0=h_[:], in1=y_[:], op=ALU.add), r=[h_, y_], w=[h_])
                OP("act", lambda e: e.activation(out=jk[:], in_=h_[:], func=AF.Square, accum_out=tv[:, 0:1]), r=[h_], w=[jk, tv])
                yield
                rstd_ss(tv, tv[:, 0:1], tv[:, 1:2], D)
                yield
                OP("dve", lambda e: e.scalar_tensor_tensor(out=o_[:], in0=h_[:], scalar=tv[:, 1:2], in1=fnwb[:], op0=ALU.mult, op1=ALU.mult),
                   r=[h_, tv, fnwb], w=[o_])
                fw.dma("sp", lambda q: q.dma_start(out=out_d[rows, :], in_=o_[:]), r=[o_], w=[OUT])

            run_pipelined(final_gen, list(range(NTILE)), nslots=NF, gap=1)
            fw.barrier()

    except _Stop:
        fw.barrier()
        return nc, fw
    root.close()
    return nc, fw


_WIN_PERM = np.concatenate([np.arange(0, 1536), np.arange(1568, 2080), np.arange(1536, 1568)])


def make_in_maps(x, c, ctx, c_ctx, w_ada, b_ada, norm_mix_w, norm_mlp_w, w_in, w_gk_f, b_gk_f, w_gk_b, b_gk_b,
                 gla_norm_w, w_pool, pool_scale, w_out, w_router, b_router, w_gu, b_gu, w_down, b_down, final_norm_w):
    f = lambda a: np.ascontiguousarray(np.asarray(a, dtype=np.float32))
    cf, cbf = _consts()
    wgk = np.zeros((32, 4, 2, 64), np.float32)
    wgk[0:16, :, 0, :] = f(w_gk_f)[0].reshape(16, 4, 64)
    wgk[16:32, :, 1, :] = f(w_gk_b)[0].reshape(16, 4, 64)
    bgk = np.stack([f(b_gk_f)[0].reshape(4, 64), f(b_gk_b)[0].reshape(4, 64)], axis=1).reshape(1, 512)
    rep = lambda v: np.ascontiguousarray(np.broadcast_to(f(v).reshape(1, -1), (128, f(v).size)))
    shared = {
        "w_ada": f(w_ada)[0], "b_ada": f(b_ada)[0].reshape(1, -1),
        "nmwT": np.ascontiguousarray(f(norm_mix_w)[0].reshape(8, 128).T), "nmlp_bc": rep(norm_mlp_w), "fnw_bc": rep(final_norm_w),
        "w_in": np.ascontiguousarray(f(w_in)[0][:, _WIN_PERM]), "wgk": wgk.reshape(32, 512), "bgk": np.ascontiguousarray(bgk),
        "gnw_bc": rep(np.tile(f(gla_norm_w)[0], 4)), "w_pool": f(w_pool)[0], "psc_bc": rep(pool_scale),
        "w_out": f(w_out)[0], "w_router": f(w_router)[0], "br_bc": rep(b_router),
        "w_gu": f(w_gu)[0], "b_gu": f(b_gu)[0], "w_down": f(w_down)[0], "b_down": f(b_down)[0],
        "cf": cf, "cb": cbf, "linit": _list_init(),
    }
    x = f(x); ctx = f(ctx); c = f(c); c_ctx = f(c_ctx)
    maps = []
    for i in range(NCORES):
        cv = np.stack([c[2 * i], c[2 * i + 1], c_ctx], axis=0)
        cT = np.ascontiguousarray(cv.reshape(3, 8, 128).transpose(2, 1, 0))
        m = dict(shared)
        m["x"] = x[2 * i:2 * i + 2].reshape(NT, D)
        m["ctx"] = ctx[2 * i:2 * i + 2].reshape(NCTX, D)
        m["cT"] = cT
        maps.append(m)
    return maps


def kernel(**inputs):
    nc, _ = build_program()
    maps = make_in_maps(**inputs)
    res = run_bass_kernel_spmd(nc, maps, core_ids=list(range(NCORES)))
    out = np.stack([np.asarray(r["out"]).reshape(2, 2048, D) for r in res.results], axis=0)
    return out.reshape(16, 2048, D).astype(np.float32)
```

```python
import numpy as np
import concourse.bass as bass
import concourse.mybir as mybir
from concourse.bass_utils import run_bass_kernel_spmd

F32 = mybir.dt.float32
BF16 = mybir.dt.bfloat16
I32 = mybir.dt.int32
ALU = mybir.AluOpType
AF = mybir.ActivationFunctionType
ET = mybir.EngineType

NCORES = 8
D = 1024
NT = 4096
NTILE = 32
NCTX = 512
E = 32
NBMAX = 32
CAP = NBMAX * 128
EPS = 1e-6
POOL_W = (2, 4, 8, 16)


class Buf:
    __slots__ = ("w", "r")

    def __init__(self):
        self.w = None
        self.r = {}


class T:
    def __init__(self, t):
        self.t = t
        self.b = Buf()

    def __getitem__(self, k):
        return self.t[k]


class FW:
    ENG = ("pe", "act", "dve", "pool", "sp")

    def __init__(self, nc, n_dma_sems=(14, 10)):
        self.nc = nc
        self.eng = {"pe": nc.tensor, "act": nc.scalar, "dve": nc.vector, "pool": nc.gpsimd, "sp": nc.sync}
        self.sem = {k: nc.alloc_semaphore(f"s_{k}") for k in self.ENG}
        self.cnt = {k: 0 for k in self.ENG}
        tot = sum(n_dma_sems)
        self.dsem = [nc.alloc_semaphore(f"d_{i}") for i in range(tot)]
        self.dcnt = [0] * tot
        self.dpool = {}
        self.downer = {}
        o = 0
        for q, n in zip(("sp", "pool"), n_dma_sems):
            self.dpool[q] = [list(range(o, o + n)), 0]
            for i in range(o, o + n):
                self.downer[i] = q
            o += n
        self.seen = {k: {} for k in self.ENG}
        self.if_stack = []
        self.n_ins = 0

    def _semh(self, key):
        return self.sem[key] if isinstance(key, str) else self.dsem[key]

    def _deps(self, reads, writes):
        deps = {}

        def add(k, v):
            if deps.get(k, 0) < v:
                deps[k] = v
        for t in reads:
            if t.b.w is not None:
                add(*t.b.w)
        for t in writes:
            if t.b.w is not None:
                add(*t.b.w)
            for k, v in t.b.r.items():
                add(k, v)
        return deps

    def _need(self, e, deps, skip_self=False):
        need = []
        for k, v in deps.items():
            if skip_self and k == e:
                continue
            if self.seen[e].get(k, 0) < v:
                need.append((k, v))
                self.seen[e][k] = v
        return need

    def op(self, e, fn, r=(), w=()):
        need = self._need(e, self._deps(r, w), skip_self=(e == "pe"))
        eng = self.eng[e]
        for k, v in need[1:]:
            eng.wait_ge(self._semh(k), v)
        ins = fn(eng)
        if need:
            ins._wait_ge(self._semh(need[0][0]), need[0][1])
        self.cnt[e] += 1
        ins.then_inc(self.sem[e], 1)
        c = self.cnt[e]
        for t in w:
            t.b.w = (e, c)
            t.b.r = {}
        for t in r:
            if t.b.r.get(e, 0) < c:
                t.b.r[e] = c
        self.n_ins += 1
        return ins

    def dma(self, q, fn, r=(), w=()):
        deps = self._deps(r, w)
        pl = self.dpool[q]
        si = pl[0][pl[1]]
        pl[1] = (pl[1] + 1) % len(pl[0])
        if self.dcnt[si] > 0:
            deps[si] = max(deps.get(si, 0), self.dcnt[si])
        need = self._need(q, deps)
        eng = self.eng[q]
        for k, v in need[1:]:
            eng.wait_ge(self._semh(k), v)
        ins = fn(eng)
        if need:
            ins._wait_ge(self._semh(need[0][0]), need[0][1])
        self.dcnt[si] += 16
        ins.then_inc(self.dsem[si], 16)
        v = self.dcnt[si]
        for t in w:
            t.b.w = (si, v)
            t.b.r = {}
        for t in r:
            t.b.r[si] = v
        self.n_ins += 1
        return ins

    def sync_to(self, e, reads):
        for k, v in self._need(e, self._deps(reads, ())):
            self.eng[e].wait_ge(self._semh(k), v)

    def wait_all(self, e):
        eng = self.eng[e]
        for k in self.ENG:
            if k != e and self.cnt[k] > self.seen[e].get(k, 0):
                eng.wait_ge(self.sem[k], self.cnt[k])
                self.seen[e][k] = self.cnt[k]
        for i, v in enumerate(self.dcnt):
            if v > self.seen[e].get(i, 0):
                eng.wait_ge(self.dsem[i], v)
                self.seen[e][i] = v

    def barrier(self):
        for e in self.ENG:
            self.wait_all(e)

    def begin_if_cmp(self, regs, val, op):
        st = dict(cnt=dict(self.cnt), dcnt=list(self.dcnt), seen={k: dict(v) for k, v in self.seen.items()})
        g = self.nc.If_cmp(regs, val, op)
        g.__enter__()
        st["g"] = g
        self.if_stack.append(st)

    def end_if(self):
        st = self.if_stack.pop()
        st["g"].__exit__(None, None, None)
        g = self.nc.Else()
        g.__enter__()
        for e in self.ENG:
            eng = self.eng[e]
            d = self.cnt[e] - st["cnt"][e]
            if d > 0:
                eng.wait_ge(self.sem[e], st["cnt"][e])
                eng.sem_inc(self.sem[e], d)
            for si in range(len(self.dsem)):
                dd = self.dcnt[si] - st["dcnt"][si]
                if dd > 0 and self.downer[si] == e:
                    eng.wait_ge(self.dsem[si], st["dcnt"][si])
                    eng.sem_inc(self.dsem[si], dd)
        g.__exit__(None, None, None)
        self.seen = st["seen"]


def _window(pos, w, length):
    lo = w // 2
    hi = w - 1 - lo
    return np.clip(pos - lo, 0, length), np.clip(pos + hi + 1, 0, length)


def _pool_consts():
    mats, index, keymap = [], {}, {}
    invcnt = np.zeros((128, 64), np.float32)
    p = np.arange(128)
    for g, w in enumerate(POOL_W):
        lo = w // 2
        hi = w - 1 - lo
        for i in range(16):
            rt = 2 * i + p // 64
            ct = p % 64
            r0, r1 = _window(rt, w, 32)
            c0, c1 = _window(ct, w, 64)
            cnt = ((r1 - r0) * (c1 - c0)).astype(np.float32)
            invcnt[:, i * 4 + g] = 1.0 / cnt
            for j in range(16):
                rs = 2 * j + p // 64
                cs = p % 64
                m = ((rs[:, None] >= rt[None, :] - lo) & (rs[:, None] <= rt[None, :] + hi) &
                     (cs[:, None] >= ct[None, :] - lo) & (cs[:, None] <= ct[None, :] + hi)).astype(np.float32)
                if not m.any():
                    continue
                if i == j:
                    m = m - np.diag(cnt)
                key = m.tobytes()
                if key not in keymap:
                    keymap[key] = len(mats)
                    mats.append(m)
                index[(g, i, j)] = keymap[key]
    return mats, index, invcnt


_POOL_MATS, _POOL_INDEX, _INVCNT = _pool_consts()
NPM = len(_POOL_MATS)

CF_ID, CF_M1F, CF_M1B, CF_INV, CF_NEG, CF_ONES, CF_SENT, CF_ECAP, CF_TOK = 0, 128, 256, 384, 448, 449, 577, 578, 610
CF_N = 611
CB_ID, CB_MF, CB_MB, CB_SU, CB_ONES, CB_POOL = 0, 128, 256, 384, 512, 640
CB_N = CB_POOL + 128 * NPM


def _consts():
    import ml_dtypes
    s = np.arange(128)[:, None]
    t = np.arange(128)[None, :]
    cf = np.zeros((128, CF_N), np.float32)
    cf[:, CF_ID:CF_ID + 128] = np.eye(128)
    cf[:, CF_M1F:CF_M1F + 128] = (s > t) / 16.0
    cf[:, CF_M1B:CF_M1B + 128] = (s < t) / 16.0
    cf[:, CF_INV:CF_INV + 64] = _INVCNT
    cf[:, CF_NEG] = -1.0 / 16.0
    cf[:, CF_ONES:CF_ONES + 128] = 1.0
    cf[:, CF_SENT] = NT + np.arange(128)
    cf[:, CF_ECAP:CF_ECAP + 32] = (np.arange(32) * CAP)[None, :]
    cf[:, CF_TOK] = np.arange(128)
    cb = np.zeros((128, CB_N), np.float32)
    cb[:, CB_ID:CB_ID + 128] = np.eye(128)
    cb[:, CB_MF:CB_MF + 128] = (s <= t)
    cb[:, CB_MB:CB_MB + 128] = (s >= t)
    cb[:, CB_SU:CB_SU + 128] = (s < t)
    cb[:, CB_ONES:CB_ONES + 128] = 1.0
    for m, mat in enumerate(_POOL_MATS):
        cb[:, CB_POOL + 128 * m:CB_POOL + 128 * (m + 1)] = mat
    return cf, cb.astype(ml_dtypes.bfloat16)


def _list_init():
    r = np.arange(128 * E * NBMAX)
    img = np.stack([NT + r % 128, np.zeros_like(r)], axis=1).astype(np.int32)
    return np.ascontiguousarray(img.reshape(128, E * NBMAX * 2))


class _Stop(Exception):
    pass


def build_program(nbmax=NBMAX, phases=4, stop=None, debug=False):
    from contextlib import ExitStack

    def CHK(name):
        if stop == name:
            raise _Stop()

    nc = bass.Bass("TRN2", target_bir_lowering=False)
    fw = FW(nc)
    OP = fw.op

    def din(name, shape, dt=F32):
        return nc.dram_tensor(name, list(shape), dt, kind="ExternalInput").ap()

    x_d = din("x", [NT, D]); ctx_d = din("ctx", [NCTX, D]); cT_d = din("cT", [128, 8, 3])
    wada_d = din("w_ada", [D, 6 * D]); bada_d = din("b_ada", [1, 6 * D])
    nmw_d = din("nmwT", [128, 8]); nmlp_d = din("nmlp_bc", [128, D]); fnw_d = din("fnw_bc", [128, D])
    win_d = din("w_in", [D, 2080]); wgk_d = din("wgk", [32, 512]); bgk_d = din("bgk", [1, 512])
    gnw_d = din("gnw_bc", [128, 512]); wpool_d = din("w_pool", [4, 128, 128]); psc_d = din("psc_bc", [128, 512])
    wout_d = din("w_out", [D, D]); wr_d = din("w_router", [D, E]); br_d = din("br_bc", [128, E])
    wgu_d = din("w_gu", [E, D, 2 * D]); bgu_d = din("b_gu", [E, 2 * D]); wdn_d = din("w_down", [E, D, D]); bdn_d = din("b_down", [E, D])
    cf_d = din("cf", [128, CF_N]); cb_d = din("cb", [128, CB_N], BF16)
    linit_d = din("linit", [128, E * NBMAX * 2], I32)
    out_d = nc.dram_tensor("out", [NT, D], F32, kind="ExternalOutput").ap()
    SK = "ExternalOutput" if debug else "Internal"
    h1_d = nc.dram_tensor("h1s", [NT, D], F32, kind=SK).ap()
    x2_d = nc.dram_tensor("x2s", [NT + 128, D], BF16, kind=SK).ap()
    yacc_d = nc.dram_tensor("yacc", [NT + 128, D], F32, kind=SK).ap()
    lists_d = nc.dram_tensor("lists", [128 * E * NBMAX, 2], I32, kind=SK).ap()
    nblk_d = nc.dram_tensor("nblk", [1, E], I32, kind=SK).ap()
    mod_d = nc.dram_tensor("mods", [3, 6 * D], F32, kind=SK).ap()
    H1D, X2D, YACC, LISTS, NBLKD, MODD = (T(None) for _ in range(6))
    wgu16_d = nc.dram_tensor("wgu16", [E, D, 2 * D], BF16).ap()
    wdn16_d = nc.dram_tensor("wdn16", [E, D, D], BF16).ap()
    WCV = [[T(None), T(None)] for _ in range(E)]
    conv_todo = [(e_, h_) for e_ in range(E) for h_ in range(2)]

    def conv_step(pace=None):
        if not conv_todo:
            return
        e_, h_ = conv_todo.pop(0)
        if pace is not None:
            fw.sync_to("pool", pace)
        if h_ == 0:
            fw.dma("pool", lambda q: q.dma_start(out=wgu16_d[e_], in_=wgu_d[e_]), w=[WCV[e_][0]])
        else:
            fw.dma("pool", lambda q: q.dma_start(out=wdn16_d[e_], in_=wdn_d[e_]), w=[WCV[e_][1]])
    dbg_d = nc.dram_tensor("dbg", [128, 8192], F32, kind=SK).ap()
    DBG = T(None)

    def DUMP(t, ap, off, n):
        fw.dma("sp", lambda q: q.dma_start(out=dbg_d[0:ap.shape[0], off:off + n], in_=ap), r=[t], w=[DBG])

    _uid = [0]

    def sb(st, name, shape, dt):
        _uid[0] += 1
        return T(st.enter_context(nc.sbuf_tensor(f"sb_{name}_{_uid[0]}", list(shape), dt)))

    root = ExitStack()
    psum = nc.alloc_psum_tensor("psum", [128, 4096], F32)
    P = [T(None) for _ in range(8)]

    def pb(i, lo=0, hi=512):
        return psum[:, i * 512 + lo:i * 512 + hi]

    def pbb(i):
        return psum[:, i * 512:(i + 1) * 512].bitcast(BF16)

    cf = sb(root, "cf", [128, CF_N], F32); cb = sb(root, "cb", [128, CB_N], BF16)
    fw.dma("sp", lambda q: q.dma_start(out=cf[:], in_=cf_d), w=[cf])
    fw.dma("sp", lambda q: q.dma_start(out=cb[:], in_=cb_d), w=[cb])
    ident = cf[:, CF_ID:CF_ID + 128]
    wi16_d = nc.dram_tensor("win16", [D, 2080], BF16).ap()
    wo16_d = nc.dram_tensor("wout16", [D, D], BF16).ap()
    WI16, WO16 = T(None), T(None)
    fw.dma("pool", lambda q: q.dma_start(out=wi16_d.rearrange("d (t n) -> (d t) n", n=1040), in_=win_d.rearrange("d (t n) -> (d t) n", n=1040)), w=[WI16])
    fw.dma("pool", lambda q: q.dma_start(out=wo16_d, in_=wout_d), w=[WO16])
    identb = cb[:, CB_ID:CB_ID + 128]
    a1fm = sb(root, "a1fm", [128, 3, 8], F32); b1fm = sb(root, "b1fm", [128, 3, 8], F32)
    carry = sb(root, "carry", [128, E], F32)
    OP("pool", lambda e: e.memset(carry[:], 0.0), w=[carry])

    def rstd_from_ss(ss, rs, n):
        OP("dve", lambda e: e.tensor_scalar(out=rs, in0=ss, scalar1=1.0 / n, scalar2=EPS, op0=ALU.mult, op1=ALU.add), r=[tmpv], w=[tmpv])
        OP("act", lambda e: e.activation(out=rs, in_=rs, func=AF.Sqrt), r=[tmpv], w=[tmpv])
        OP("dve", lambda e: e.reciprocal(out=rs, in_=rs), r=[tmpv], w=[tmpv])

    tmpv = sb(root, "tmpv", [128, 64], F32)
    z4 = sb(root, "z4", [128, 4], F32)
    nbi = sb(root, "nbi", [128, E], I32)
    OP("pool", lambda e: e.memset(z4[:], 0.0), w=[z4])

    try:
        with ExitStack() as st:
            cTs = sb(st, "cTs", [128, 8, 3], F32)
            wa = [sb(st, f"wa{i}", [128, 8, 512], F32) for i in range(2)]
            badab = sb(st, "badab", [3, 6 * D], F32)
            modrow = sb(st, "modrow", [3, 6 * D], F32)
            sfm = sb(st, "sfm", [128, 3, 8], F32)
            nmw = sb(st, "nmw", [128, 8], F32)
            fw.dma("sp", lambda q: q.dma_start(out=cTs[:], in_=cT_d), w=[cTs])
            fw.dma("sp", lambda q: q.dma_start(out=badab[:], in_=bada_d.partition_broadcast(3)), w=[badab])
            fw.dma("sp", lambda q: q.dma_start(out=nmw[:], in_=nmw_d), w=[nmw])
            OP("act", lambda e: e.activation(out=cTs[:], in_=cTs[:], func=AF.Silu), r=[cTs], w=[cTs])
            wav = wada_d.rearrange("(k p) n -> p k n", p=128)
            for n in range(12):
                wt = wa[n % 2]
                fw.dma("sp", lambda q: q.dma_start(out=wt[:], in_=wav[:, :, n * 512:(n + 1) * 512]), w=[wt])
                for k in range(8):
                    OP("pe", lambda e: e.matmul(pb(0)[0:3, :], lhsT=cTs[:, k, :], rhs=wt[:, k, :], start=(k == 0), stop=(k == 7)),
                       r=[cTs, wt], w=[P[0]])
                OP("dve", lambda e: e.tensor_tensor(out=modrow[0:3, n * 512:(n + 1) * 512], in0=pb(0)[0:3, :],
                                                    in1=badab[0:3, n * 512:(n + 1) * 512], op=ALU.add), r=[P[0], badab], w=[modrow])
            fw.dma("sp", lambda q: q.dma_start(out=mod_d, in_=modrow[0:3, :]), r=[modrow], w=[MODD])
            for j in range(3):
                fw.dma("sp", lambda q: q.dma_start(out=b1fm[:, j, :], in_=mod_d[j, 0:D].rearrange("(c p) -> p c", p=128),
                                                   allow_slow_non_contiguous=True), r=[MODD], w=[b1fm])
                fw.dma("sp", lambda q: q.dma_start(out=sfm[:, j, :], in_=mod_d[j, D:2 * D].rearrange("(c p) -> p c", p=128),
                                                   allow_slow_non_contiguous=True), r=[MODD], w=[sfm])
                OP("dve", lambda e: e.scalar_tensor_tensor(out=a1fm[:, j, :], in0=sfm[:, j, :], scalar=1.0, in1=nmw[:],
                                                           op0=ALU.add, op1=ALU.mult), r=[sfm, nmw], w=[a1fm])
            fw.barrier()
        CHK("p0")

        with ExitStack() as st:
            wpoolb = sb(st, "wpoolb", [128, 4, 128], BF16)
            wrb = sb(st, "wrb", [128, 8, E], BF16)
            wgk = sb(st, "wgk", [32, 512], F32); bgk = sb(st, "bgk", [1, 512], F32)
            gnwb = sb(st, "gnwb", [128, 512], F32); pscb = sb(st, "pscb", [128, 512], F32)
            brb = sb(st, "brb", [128, E], F32)
            fw.dma("pool", lambda q: q.dma_start(out=wpoolb[:], in_=wpool_d.rearrange("g c d -> c g d")), w=[wpoolb])
            fw.dma("pool", lambda q: q.dma_start(out=wrb[:], in_=wr_d.rearrange("(k p) n -> p k n", p=128)), w=[wrb])
            for tt, dd in ((wgk, wgk_d), (bgk, bgk_d), (gnwb, gnw_d), (pscb, psc_d), (brb, br_d)):
                fw.dma("sp", lambda q: q.dma_start(out=tt[:], in_=dd), w=[tt])
            fw.dma("sp", lambda q: q.dma_start(out=lists_d.rearrange("(q r) two -> q (r two)", q=128), in_=linit_d), w=[LISTS])
            zero_todo = []
            CHK("init")

            vS = [sb(st, f"vS{i}", [128, 512], BF16) for i in range(18)]
            kdS = [sb(st, f"kdS{i}", [128, 512], BF16) for i in range(18)]
            decS = [sb(st, f"decS{i}", [128, 4], F32) for i in range(18)]
            gwS = [sb(st, f"gwS{i}", [128, 512], BF16) for i in range(16)]
            plS = [sb(st, f"plS{i}", [128, 512], BF16) for i in range(16)]
            qTS = [sb(st, f"qTS{i}", [128, 4, 128], BF16) for i in range(16)]
            kTS = [sb(st, f"kTS{i}", [128, 4, 128], BF16) for i in range(16)]
            dSS = [sb(st, f"dSS{i}", [128, 4, 128], BF16) for i in range(16)]
            S = sb(st, "S", [128, 4, 128], F32)
            ones1 = cf[0:1, CF_ONES:CF_ONES + 128]
            SLOT_BANKS = ((0, 2, 4, 6), (1, 3, 5, 7))

            def rstd_ss(tv, ss, rs, n):
                OP("dve", lambda e: e.tensor_scalar(out=rs, in0=ss, scalar1=1.0 / n, scalar2=EPS, op0=ALU.mult, op1=ALU.add), r=[tv], w=[tv])
                OP("act", lambda e: e.activation(out=rs, in_=rs, func=AF.Sqrt), r=[tv], w=[tv])
                OP("dve", lambda e: e.reciprocal(out=rs, in_=rs), r=[tv], w=[tv])

            def run_pipelined(make_gen, items, nslots=2, gap=3):
                items = list(items)
                active = []
                since = gap
                while items or active:
                    if items and len(active) < nslots and since >= gap:
                        used = {s_ for s_, _ in active}
                        slot = [s_ for s_ in range(nslots) if s_ not in used][0]
                        active.append((slot, make_gen(items.pop(0), slot)))
                        since = 0
                    for ent in list(active):
                        try:
                            next(ent[1])
                        except StopIteration:
                            active.remove(ent)
                    since += 1

            def u_mm(si, bank):
                for h in range(4):
                    OP("pe", lambda e: e.matmul(pb(bank, h * 128, (h + 1) * 128), lhsT=kdS[si][:, h * 128:(h + 1) * 128],
                                                rhs=vS[si][:, h * 128:(h + 1) * 128], start=True, stop=True), r=[kdS[si], vS[si]], w=[P[bank]])

            def state_update(si, bank, lo, hi, store=None):
                for h in range(4):
                    if store is not None:
                        OP("act", lambda e: e.activation(out=store[lo:hi, h, :], in_=S[lo:hi, h, :], func=AF.Copy, scale=decS[si][lo:hi, h:h + 1]),
                           r=[S, decS[si]], w=[store])
                    OP("dve", lambda e: e.scalar_tensor_tensor(out=S[lo:hi, h, :], in0=S[lo:hi, h, :], scalar=decS[si][lo:hi, h:h + 1],
                                                               in1=pb(bank, h * 128, (h + 1) * 128)[lo:hi, :], op0=ALU.mult, op1=ALU.add),
                       r=[S, decS[si], P[bank]], w=[S])

            for b in range(2):
              with ExitStack() as sp_:
                winb = sb(sp_, "winb", [128, 8, 2080], BF16)
                fw.dma("sp", lambda q: q.dma_start(out=winb[:], in_=wi16_d.rearrange("(k p) n -> p k n", p=128)), r=[WI16], w=[winb])
                W = []
                for s_ in range(2):
                    W.append(dict(
                        xmT=sb(sp_, f"xmT{s_}", [128, 8, 128], BF16),
                        qk=sb(sp_, f"qk{s_}", [128, 512], F32), rsb=sb(sp_, f"rsb{s_}", [128, 32], F32), rT=sb(sp_, f"rT{s_}", [32, 128], F32),
                        Lt=sb(sp_, f"Lt{s_}", [128, 512], F32), Em=sb(sp_, f"Em{s_}", [128, 512], F32), qt=sb(sp_, f"qt{s_}", [128, 512], BF16),
                        junk=sb(sp_, f"junk{s_}", [128, D], BF16), tv=sb(sp_, f"tvp{s_}", [128, 8], F32)))

                xring = [sb(sp_, f"xring{i_}", [128, D], F32) for i_ in range(3)]
                xsrc = {}
                xissued = set()

                def x_issue(g):
                    if g in xissued or g not in xsrc:
                        return
                    xissued.add(g)
                    fw.dma("sp", lambda q: q.dma_start(out=xring[g % 3][:], in_=xsrc[g]), w=[xring[g % 3]])

                def prep_gen(item, slot):
                    g, src, is_ctx, si, a1, b1 = item
                    w_ = W[slot]
                    b0, b1k, b2, b3 = SLOT_BANKS[slot]
                    xmT, qk, rsb, rT, Lt, Em, qt, junk, tv = (w_[k] for k in ("xmT", "qk", "rsb", "rT", "Lt", "Em", "qt", "junk", "tv"))
                    Ep, gsil = Lt, Em
                    xi = xring[g % 3]
                    x_issue(g)
                    x_issue(g + 1)
                    OP("act", lambda e: e.activation(out=junk[:], in_=xi[:], func=AF.Square, accum_out=tv[:, 0:1]), r=[xi], w=[junk, tv])
                    rstd_ss(tv, tv[:, 0:1], tv[:, 1:2], D)
                    OP("act", lambda e: e.activation(out=xi[:], in_=xi[:], func=AF.Copy, scale=tv[:, 1:2]), r=[xi, tv], w=[xi])
                    yield
                    conv_step(pace=[xi])
                    for hf in range(2):
                        for c in range(4):
                            k = hf * 4 + c
                            OP("pe", lambda e: e.transpose(out=pb(b0, c * 128, (c + 1) * 128), in_=xi[:, k * 128:(k + 1) * 128], identity=ident),
                               r=[xi, cf], w=[P[b0]])
                        for c in range(4):
                            k = hf * 4 + c
                            OP("act", lambda e: e.activation(out=xmT[:, k, :], in_=pb(b0, c * 128, (c + 1) * 128), func=AF.Identity,
                                                             scale=a1[:, k:k + 1], bias=b1[:, k:k + 1]), r=[P[b0], a1fm, b1fm], w=[xmT])
                        yield
                    groups = [(0, 512, "qk"), (512, 1024, "v")] + ([] if is_ctx else [(1024, 1536, "g"), (1536, 2048, "pool")]) + [(2048, 2080, "r")]
                    for gi, (lo, hi, nm) in enumerate(groups):
                        bk = b1k
                        for k in range(8):
                            OP("pe", lambda e: e.matmul(pb(bk, 0, hi - lo), lhsT=xmT[:, k, :], rhs=winb[:, k, lo:hi], start=(k == 0), stop=(k == 7)),
                               r=[xmT, winb], w=[P[bk]])
                        if nm == "qk":
                            OP("dve", lambda e: e.tensor_copy(out=qk[:], in_=pb(bk)), r=[P[bk]], w=[qk])
                        elif nm == "v":
                            OP("act", lambda e: e.activation(out=vS[si][:], in_=pb(bk), func=AF.Copy), r=[P[bk]], w=[vS[si]])
                        elif nm == "g":
                            OP("act", lambda e: e.activation(out=gsil[:], in_=pb(bk), func=AF.Silu), r=[P[bk]], w=[gsil])
                            OP("dve", lambda e: e.tensor_tensor(out=gwS[si][:], in0=gsil[:], in1=gnwb[:], op=ALU.mult), r=[gsil, gnwb], w=[gwS[si]])
                        elif nm == "pool":
                            OP("dve", lambda e: e.tensor_copy(out=plS[si][:], in_=pb(bk)), r=[P[bk]], w=[plS[si]])
                        else:
                            OP("dve", lambda e: e.tensor_copy(out=rsb[:], in_=pb(bk, 0, 32)), r=[P[bk]], w=[rsb])
                        yield
                    OP("pe", lambda e: e.transpose(out=pb(b2, 0, 128)[0:32, :], in_=rsb[:, 0:32], identity=ident), r=[rsb, cf], w=[P[b2]])
                    OP("dve", lambda e: e.tensor_copy(out=rT[:], in_=pb(b2, 0, 128)[0:32, :]), r=[P[b2]], w=[rT])
                    OP("pe", lambda e: e.matmul(pb(b2), lhsT=rT[:], rhs=wgk[:], start=True, stop=False), r=[rT, wgk], w=[P[b2]])
                    OP("pe", lambda e: e.matmul(pb(b2), lhsT=ones1, rhs=bgk[:], start=False, stop=True), r=[cf, bgk], w=[P[b2]])
                    OP("act", lambda e: e.activation(out=Lt[:], in_=pb(b2), func=AF.Exp, scale=-1.0), r=[P[b2]], w=[Lt])
                    OP("act", lambda e: e.activation(out=Lt[:], in_=Lt[:], func=AF.Ln, bias=1.0), r=[Lt], w=[Lt])
                    yield
                    for h in range(4):
                        for d in range(2):
                            m1 = cf[:, (CF_M1F if d == 0 else CF_M1B):(CF_M1F if d == 0 else CF_M1B) + 128]
                            c0 = h * 128 + d * 64
                            OP("pe", lambda e: e.matmul(pb(b2, c0, c0 + 64), lhsT=m1, rhs=Lt[:, c0:c0 + 64], start=True, stop=True),
                               r=[cf, Lt], w=[P[b2]])
                    for h in range(4):
                        OP("pe", lambda e: e.matmul(pb(b3, h, h + 1), lhsT=Lt[:, h * 128:(h + 1) * 128], rhs=cf[:, CF_NEG:CF_NEG + 1],
                                                    start=True, stop=True), r=[Lt, cf], w=[P[b3]])
                    OP("act", lambda e: e.activation(out=decS[si][:], in_=pb(b3, 0, 4), func=AF.Exp), r=[P[b3]], w=[decS[si]])
                    OP("act", lambda e: e.activation(out=Em[:], in_=pb(b2), func=AF.Exp, scale=-1.0), r=[P[b2]], w=[Em])
                    k4 = qk[:, 256:512].rearrange("p (h k) -> p h k", h=4)
                    for d in range(2):
                        OP("dve", lambda e: e.tensor_tensor(
                            out=kdS[si][:].rearrange("p (h d k) -> p h d k", h=4, d=2)[:, :, d, :], in0=k4,
                            in1=Em[:].rearrange("p (h d k) -> p h d k", h=4, d=2)[:, :, d, :], op=ALU.mult), r=[qk, Em], w=[kdS[si]])
                    yield
                    if is_ctx:
                        u_mm(si, b3)
                        return
                    OP("act", lambda e: e.activation(out=Ep[:], in_=pb(b2), func=AF.Exp), r=[P[b2]], w=[Ep])
                    q4 = qk[:, 0:256].rearrange("p (h k) -> p h k", h=4)
                    for d in range(2):
                        OP("dve", lambda e: e.scalar_tensor_tensor(
                            out=qt[:].rearrange("p (h d k) -> p h d k", h=4, d=2)[:, :, d, :], in0=q4, scalar=0.125,
                            in1=Ep[:].rearrange("p (h d k) -> p h d k", h=4, d=2)[:, :, d, :], op0=ALU.mult, op1=ALU.mult), r=[qk, Ep], w=[qt])
                    yield
                    for srcT, dstT, bk, eng in ((qt, qTS[si], b0, "act"), (kdS[si], kTS[si], b1k, "dve")):
                        for h in range(4):
                            OP("pe", lambda e: e.transpose(out=pbb(bk)[:, h * 128:(h + 1) * 128], in_=srcT[:, h * 128:(h + 1) * 128], identity=identb),
                               r=[srcT, cb], w=[P[bk]])
                        if eng == "act":
                            OP("act", lambda e: e.activation(out=dstT[:].rearrange("p h t -> p (h t)"), in_=pbb(bk)[:, 0:512], func=AF.Copy), r=[P[bk]], w=[dstT])
                        else:
                            OP("dve", lambda e: e.tensor_copy(out=dstT[:].rearrange("p h t -> p (h t)"), in_=pbb(bk)[:, 0:512]), r=[P[bk]], w=[dstT])
                    yield
                    u_mm(si, b3)
                    state_update(si, b3, 0, 64, store=dSS[si])

                OP("pool", lambda e: e.memset(S[:], 0.0), w=[S])
                for j in range(2):
                    xsrc[j] = ctx_d[b * 256 + j * 128: b * 256 + (j + 1) * 128, :]
                for i in range(16):
                    xsrc[2 + i] = x_d[b * 2048 + i * 128: b * 2048 + (i + 1) * 128, :]
                run_pipelined(prep_gen, [(j, xsrc[j], True, 16 + j, a1fm[:, 2, :], b1fm[:, 2, :]) for j in range(2)], gap=0)
                for j in (0, 1):
                    state_update(16 + j, SLOT_BANKS[j][3], 0, 64)
                for j in (1, 0):
                    state_update(16 + j, SLOT_BANKS[j][3], 64, 128)
                if stop == "ctx":
                    DUMP(S, S[:].rearrange("p h v -> p (h v)"), 0, 512)
                CHK("ctx")
                run_pipelined(prep_gen, [(2 + i, xsrc[2 + i], False, i, a1fm[:, b, :], b1fm[:, b, :]) for i in range(16)])
                fw.barrier()
              CHK("prep")
              with ExitStack() as sk_:
                woutb = sb(sk_, "woutb", [128, 8, D], BF16)
                g1b = sb(sk_, "g1b", [128, D], F32); a2b = sb(sk_, "a2b", [128, D], F32); b2b = sb(sk_, "b2b", [128, D], F32)
                fw.dma("sp", lambda q: q.dma_start(out=woutb[:], in_=wo16_d.rearrange("(k p) n -> p k n", p=128)), r=[WO16], w=[woutb])
                fw.dma("sp", lambda q: q.dma_start(out=g1b[:], in_=mod_d[b:b + 1, 2 * D:3 * D].partition_broadcast(128)), r=[MODD], w=[g1b])
                fw.dma("sp", lambda q: q.dma_start(out=b2b[:], in_=nmlp_d), w=[b2b])
                fw.dma("sp", lambda q: q.dma_start(out=a2b[:], in_=mod_d[b:b + 1, 4 * D:5 * D].partition_broadcast(128)), r=[MODD], w=[a2b])
                OP("dve", lambda e: e.scalar_tensor_tensor(out=a2b[:], in0=a2b[:], scalar=1.0, in1=b2b[:], op0=ALU.add, op1=ALU.mult),
                   r=[a2b, b2b], w=[a2b])
                fw.dma("sp", lambda q: q.dma_start(out=b2b[:], in_=mod_d[b:b + 1, 3 * D:4 * D].partition_broadcast(128)), r=[MODD], w=[b2b])
                if b == 0:
                    zt = sb(sk_, "zt", [128, D], F32)
                    OP("pool", lambda e: e.memset(zt[:], 0.0), w=[zt])
                    for i_ in range(NTILE + 1):
                        zero_todo.append(lambda i_=i_: fw.dma("sp", lambda q: q.dma_start(out=yacc_d[i_ * 128:(i_ + 1) * 128, :], in_=zt[:]), r=[zt]))
                    zero_todo.append(lambda: fw.dma("sp", lambda q: q.dma_start(out=x2_d[NT:NT + 128, :], in_=zt[:, 0:512].bitcast(BF16)), r=[zt]))
                V = []
                for s_ in range(2):
                    V.append(dict(
                        xr=sb(sk_, f"xr{s_}", [128, D], F32), h1=sb(sk_, f"h1{s_}", [128, D], F32), mix=sb(sk_, f"mix{s_}", [128, D], BF16),
                        mixT=sb(sk_, f"mixT{s_}", [128, 8, 128], BF16), rawT=sb(sk_, f"rawT{s_}", [128, 512], BF16),
                        junk=sb(sk_, f"junkb{s_}", [128, D], BF16), aTf=sb(sk_, f"aTf{s_}", [128, 128], BF16), aTb=sb(sk_, f"aTb{s_}", [128, 128], BF16),
                        tv=sb(sk_, f"tvb{s_}", [128, 32], F32), lg=sb(sk_, f"lg{s_}", [128, E], F32), msk=sb(sk_, f"msk{s_}", [128, E], F32),
                        mskb=sb(sk_, f"mskb{s_}", [128, E], BF16), t8=sb(sk_, f"t8{s_}", [128, 8], F32), addr=sb(sk_, f"addr{s_}", [128, E], F32),
                        oh=sb(sk_, f"oh{s_}", [128, E], F32), dst=sb(sk_, f"dst{s_}", [128, 8], F32),
                        dsi=[sb(sk_, f"dsi{s_}{r_}", [128, 4], I32) for r_ in range(4)],
                        pk=[sb(sk_, f"pk{s_}{r_}", [128, 4, 2], I32) for r_ in range(4)], e4=sb(sk_, f"e4{s_}", [128, 4], F32)))

                def back_gen(i, slot):
                    v_ = V[slot]
                    t0, t1, t2, t3 = SLOT_BANKS[slot]
                    xr, h1, mix, mixT, rawT, junk, aTf, aTb, tv = (v_[k] for k in ("xr", "h1", "mix", "mixT", "rawT", "junk", "aTf", "aTb", "tv"))
                    lg, msk, mskb, t8, addr, oh, dst, dsi, pki, e4 = (v_[k] for k in ("lg", "msk", "mskb", "t8", "addr", "oh", "dst", "dsi", "pk", "e4"))
                    dsi, pki = dsi[(i // 2) % 4], pki[(i // 2) % 4]
                    x2, x2T = mix, mixT
                    tg = b * 16 + i
                    rows = slice(tg * 128, (tg + 1) * 128)
                    u_mm(i, t0)
                    state_update(i, t0, 64, 128, store=dSS[i])
                    fw.dma("sp", lambda q: q.dma_start(out=xr[:], in_=x_d[rows, :]), w=[xr])
                    yield
                    conv_step(pace=[xr])
                    for _ in range(3):
                        if zero_todo:
                            zero_todo.pop(0)()
                    for h in range(4):
                        OP("pe", lambda e: e.matmul(pb(t1, 0, 128), lhsT=kTS[i][0:64, h, :], rhs=qTS[i][0:64, h, :], start=True, stop=True),
                           r=[kTS[i], qTS[i]], w=[P[t1]])
                        OP("pe", lambda e: e.matmul(pb(t2, 0, 128), lhsT=kTS[i][64:128, h, :], rhs=qTS[i][64:128, h, :], start=True, stop=True),
                           r=[kTS[i], qTS[i]], w=[P[t2]])
                        OP("dve", lambda e: e.tensor_tensor(out=aTf[:], in0=pb(t1, 0, 128), in1=cb[:, CB_MF:CB_MF + 128], op=ALU.mult), r=[P[t1], cb], w=[aTf])
                        OP("dve", lambda e: e.tensor_tensor(out=aTb[:], in0=pb(t2, 0, 128), in1=cb[:, CB_MB:CB_MB + 128], op=ALU.mult), r=[P[t2], cb], w=[aTb])
                        o_ps = pb(t3, h * 128, (h + 1) * 128)
                        vh = vS[i][:, h * 128:(h + 1) * 128]
                        OP("pe", lambda e: e.matmul(o_ps, lhsT=aTf[:], rhs=vh, start=True, stop=False), r=[aTf, vS[i]], w=[P[t3]])
                        OP("pe", lambda e: e.matmul(o_ps, lhsT=aTb[:], rhs=vh, start=False, stop=False), r=[aTb, vS[i]], w=[P[t3]])
                        OP("pe", lambda e: e.matmul(o_ps, lhsT=qTS[i][:, h, :], rhs=dSS[i][:, h, :], start=False, stop=True), r=[qTS[i], dSS[i]], w=[P[t3]])
                        yield
                    for h in range(4):
                        OP("act", lambda e: e.activation(out=junk[:, h * 128:(h + 1) * 128], in_=pb(t3, h * 128, (h + 1) * 128), func=AF.Square,
                                                         accum_out=tv[:, 8 + h:9 + h]), r=[P[t3]], w=[junk, tv])
                    rstd_ss(tv, tv[:, 8:12], tv[:, 12:16], 128)
                    for h in range(4):
                        OP("dve", lambda e: e.scalar_tensor_tensor(out=mix[:, h * 128:(h + 1) * 128], in0=pb(t3, h * 128, (h + 1) * 128),
                                                                   scalar=tv[:, 12 + h:13 + h], in1=gwS[i][:, h * 128:(h + 1) * 128],
                                                                   op0=ALU.mult, op1=ALU.mult), r=[P[t3], tv, gwS[i]], w=[mix])
                    yield
                    for g in range(4):
                        js = [j for j in range(16) if (g, i, j) in _POOL_INDEX]
                        for n, j in enumerate(js):
                            m = _POOL_INDEX[(g, i, j)]
                            OP("pe", lambda e: e.matmul(pb(t0, g * 128, (g + 1) * 128), lhsT=plS[j][:, g * 128:(g + 1) * 128],
                                                        rhs=cb[:, CB_POOL + m * 128:CB_POOL + (m + 1) * 128], start=(n == 0), stop=(n == len(js) - 1)),
                               r=[plS[j], cb], w=[P[t0]])
                    OP("act", lambda e: e.activation(out=rawT[:], in_=pb(t0), func=AF.Copy), r=[P[t0]], w=[rawT])
                    yield
                    for g in range(4):
                        OP("pe", lambda e: e.matmul(pb(t1, g * 128, (g + 1) * 128), lhsT=rawT[:, g * 128:(g + 1) * 128], rhs=wpoolb[:, g, :],
                                                    start=True, stop=True), r=[rawT, wpoolb], w=[P[t1]])
                    for g in range(4):
                        OP("dve", lambda e: e.scalar_tensor_tensor(out=mix[:, 512 + g * 128:512 + (g + 1) * 128], in0=pb(t1, g * 128, (g + 1) * 128),
                                                                   scalar=cf[:, CF_INV + i * 4 + g:CF_INV + i * 4 + g + 1],
                                                                   in1=pscb[:, g * 128:(g + 1) * 128], op0=ALU.mult, op1=ALU.mult),
                           r=[P[t1], cf, pscb], w=[mix])
                    yield
                    for k in range(8):
                        OP("pe", lambda e: e.transpose(out=pbb(t2)[:, k * 128:(k + 1) * 128], in_=mix[:, k * 128:(k + 1) * 128], identity=identb),
                           r=[mix, cb], w=[P[t2]])
                    OP("act", lambda e: e.activation(out=mixT[:].rearrange("p k t -> p (k t)"), in_=pbb(t2), func=AF.Copy), r=[P[t2]], w=[mixT])
                    yield
                    for n, bk in enumerate((t0, t1)):
                        for k in range(8):
                            OP("pe", lambda e: e.matmul(pb(bk), lhsT=mixT[:, k, :], rhs=woutb[:, k, n * 512:(n + 1) * 512], start=(k == 0), stop=(k == 7)),
                               r=[mixT, woutb], w=[P[bk]])
                        OP("dve", lambda e: e.tensor_tensor(out=h1[:, n * 512:(n + 1) * 512], in0=pb(bk), in1=g1b[:, n * 512:(n + 1) * 512], op=ALU.mult),
                           r=[P[bk], g1b], w=[h1])
                    OP("dve", lambda e: e.tensor_tensor(out=h1[:], in0=h1[:], in1=xr[:], op=ALU.add), r=[h1, xr], w=[h1])
                    fw.dma("sp", lambda q: q.dma_start(out=h1_d[rows, :], in_=h1[:]), r=[h1], w=[H1D])
                    yield
                    OP("act", lambda e: e.activation(out=junk[:], in_=h1[:], func=AF.Square, accum_out=tv[:, 16:17]), r=[h1], w=[junk, tv])
                    rstd_ss(tv, tv[:, 16:17], tv[:, 17:18], D)
                    OP("dve", lambda e: e.scalar_tensor_tensor(out=xr[:], in0=h1[:], scalar=tv[:, 17:18], in1=a2b[:], op0=ALU.mult, op1=ALU.mult),
                       r=[h1, tv, a2b], w=[xr])
                    OP("dve", lambda e: e.tensor_tensor(out=x2[:], in0=xr[:], in1=b2b[:], op=ALU.add), r=[xr, b2b], w=[x2])
                    fw.dma("sp", lambda q: q.dma_start(out=x2_d[rows, :], in_=x2[:]), r=[x2], w=[X2D])
                    yield
                    for k in range(8):
                        OP("pe", lambda e: e.transpose(out=pbb(t2)[:, k * 128:(k + 1) * 128], in_=x2[:, k * 128:(k + 1) * 128], identity=identb),
                           r=[x2, cb], w=[P[t2]])
                    OP("act", lambda e: e.activation(out=x2T[:].rearrange("p k t -> p (k t)"), in_=pbb(t2), func=AF.Copy), r=[P[t2]], w=[x2T])
                    for k in range(8):
                        OP("pe", lambda e: e.matmul(pb(t3, 0, E), lhsT=x2T[:, k, :], rhs=wrb[:, k, :], start=(k == 0), stop=(k == 7)), r=[x2T, wrb], w=[P[t3]])
                    OP("dve", lambda e: e.tensor_tensor(out=lg[:], in0=pb(t3, 0, E), in1=brb[:], op=ALU.add), r=[P[t3], brb], w=[lg])
                    OP("dve", lambda e: e.max(out=t8[:], in_=lg[:]), r=[lg], w=[t8])
                    OP("dve", lambda e: e.tensor_scalar(out=msk[:], in0=lg[:], scalar1=t8[:, 3:4], scalar2=None, op0=ALU.is_ge), r=[lg, t8], w=[msk])
                    OP("dve", lambda e: e.tensor_copy(out=mskb[:], in_=msk[:]), r=[msk], w=[mskb])
                    yield
                    OP("dve", lambda e: e.tensor_scalar(out=tv[:, 20:21], in0=t8[:, 0:1], scalar1=-1.0, scalar2=None, op0=ALU.mult), r=[t8, tv], w=[tv])
                    OP("act", lambda e: e.activation(out=e4[:], in_=t8[:, 0:4], func=AF.Exp, bias=tv[:, 20:21], accum_out=tv[:, 21:22]),
                       r=[t8, tv], w=[e4, tv])
                    OP("dve", lambda e: e.reciprocal(out=tv[:, 22:23], in_=tv[:, 21:22]), r=[tv], w=[tv])
                    OP("dve", lambda e: e.tensor_scalar(out=e4[:], in0=e4[:], scalar1=tv[:, 22:23], scalar2=None, op0=ALU.mult), r=[e4, tv], w=[e4])
                    OP("pe", lambda e: e.matmul(pb(t3, 64, 64 + E), lhsT=cb[:, CB_SU:CB_SU + 128], rhs=mskb[:], start=True, stop=True), r=[cb, mskb], w=[P[t3]])
                    OP("pe", lambda e: e.matmul(pb(t3, 128, 128 + E), lhsT=cb[:, CB_ONES:CB_ONES + 128], rhs=mskb[:], start=True, stop=True), r=[cb, mskb], w=[P[t3]])
                    OP("dve", lambda e: e.tensor_tensor(out=addr[:], in0=pb(t3, 64, 64 + E), in1=carry[:], op=ALU.add), r=[P[t3], carry], w=[addr])
                    OP("dve", lambda e: e.tensor_tensor(out=carry[:], in0=pb(t3, 128, 128 + E), in1=carry[:], op=ALU.add), r=[P[t3], carry], w=[carry])
                    OP("dve", lambda e: e.tensor_tensor(out=addr[:], in0=addr[:], in1=cf[:, CF_ECAP:CF_ECAP + E], op=ALU.add), r=[addr, cf], w=[addr])
                    for k in range(4):
                        OP("dve", lambda e: e.tensor_scalar(out=oh[:], in0=lg[:], scalar1=t8[:, k:k + 1], scalar2=None, op0=ALU.is_equal), r=[lg, t8], w=[oh])
                        OP("dve", lambda e: e.tensor_tensor(out=oh[:], in0=oh[:], in1=addr[:], op=ALU.mult), r=[oh, addr], w=[oh])
                        OP("dve", lambda e: e.reduce_sum(out=dst[:, k:k + 1], in_=oh[:], axis=mybir.AxisListType.X), r=[oh], w=[dst])
                    OP("dve", lambda e: e.tensor_copy(out=dsi[:], in_=dst[:, 0:4]), r=[dst], w=[dsi])
                    OP("dve", lambda e: e.tensor_scalar(out=pki[:, :, 0], in0=z4[:], scalar1=cf[:, CF_TOK:CF_TOK + 1], scalar2=float(tg * 128),
                                                        op0=ALU.add, op1=ALU.add), r=[z4, cf], w=[pki])
                    OP("dve", lambda e: e.tensor_copy(out=pki[:, :, 1], in_=e4[:].bitcast(I32)), r=[e4], w=[pki])
                    for k in range(4):
                        fw.dma("pool", lambda q: q.indirect_dma_start(out=lists_d, out_offset=bass.IndirectOffsetOnAxis(ap=dsi[:, k:k + 1], axis=0),
                                                                      in_=pki[:, k, :], in_offset=None), r=[pki, dsi])

                run_pipelined(back_gen, list(range(15, -1, -1)))
                while zero_todo:
                    zero_todo.pop(0)()
                fw.barrier()
            nbf = sb(st, "nbf", [128, 2 * E], F32)
            OP("pool", lambda e: e.memset(nbf[:], 0.0), w=[nbf])
            for j in range(NBMAX):
                OP("dve", lambda e: e.scalar_tensor_tensor(out=nbf[:, 0:E], in0=carry[:], scalar=float(128 * j), in1=nbf[:, 0:E],
                                                           op0=ALU.is_gt, op1=ALU.add), r=[carry, nbf], w=[nbf])
            OP("dve", lambda e: e.tensor_copy(out=nbi[:], in_=nbf[:, 0:E]), r=[nbf], w=[nbi])
            fw.dma("sp", lambda q: q.dma_start(out=nblk_d, in_=nbi[0:1, :]), r=[nbi], w=[NBLKD])
            fw.barrier()

        if phases >= 3:
          with ExitStack() as st:
            wgu = [sb(st, f"wgu{i}", [128, 8, 2 * D], BF16) for i in range(2)]
            wdn = [sb(st, f"wdn{i}", [128, 8, D], BF16) for i in range(2)]
            bst = sb(st, "bst", [1, 3 * D], F32)
            ids = [sb(st, f"ids{i}", [128, 2], I32) for i in range(3)]
            xg = [sb(st, f"xg{i}", [128, D], BF16) for i in range(3)]
            xgT = [sb(st, f"xgT{i}", [128, 8, 128], BF16) for i in range(2)]
            gc = [[sb(st, f"gc{p}{h}", [128, 512], F32) for h in range(2)] for p in range(1)]
            sg = [[sb(st, f"sg{p}{h}", [128, 512], F32) for h in range(2)] for p in range(1)]
            uc = [[sb(st, f"uc{p}{h}", [128, 512], F32) for h in range(2)] for p in range(1)]
            actb = [[sb(st, f"actb{p}{h}", [128, 512], BF16) for h in range(2)] for p in range(1)]
            actT = [[sb(st, f"actT{p}{h}", [128, 4, 128], BF16) for h in range(2)] for p in range(1)]
            yw = [sb(st, f"yw{i}", [128, D], F32) for i in range(2)]
            ones1b = cb[0:1, CB_ONES:CB_ONES + 128]
            bhl = [[sb(st, f"bhl{i}{j}", [1, 3 * D], BF16) for j in range(1)] for i in range(2)]
            regs = nc.alloc_registers("nblk", [ET.PE, ET.Activation, ET.DVE, ET.Pool, ET.SP])
            PGh = [T(None), T(None)]
            PD = T(None); PT0 = P[0]
            PT1h = [T(None), T(None)]

            def load_bias(ex):
                fw.dma("sp", lambda q: q.dma_start(out=bst[0:1, 0:2 * D], in_=bgu_d[ex:ex + 1, :]), w=[bst])
                fw.dma("sp", lambda q: q.dma_start(out=bst[0:1, 2 * D:3 * D], in_=bdn_d[ex:ex + 1, :]), w=[bst])
                hi = bhl[ex % 2][0]
                OP("dve", lambda e: e.tensor_copy(out=hi[:], in_=bst[:]), r=[bst], w=[hi])

            wgh = [[T(None) for _ in range(8)] for _ in range(2)]
            wdh = [[T(None) for _ in range(8)] for _ in range(2)]

            def weight_pieces(ex):
                wg, wd = wgu[ex % 2], wdn[ex % 2]
                pcs = []
                for k in range(8):
                    pcs.append(lambda k=k: fw.dma("sp", lambda q: q.dma_start(out=wg[:, k, :], in_=wgu16_d[ex, k * 128:(k + 1) * 128, :]),
                                                  r=[WCV[ex][0]], w=[wgh[ex % 2][k]]))
                for k in range(8):
                    pcs.append(lambda k=k: fw.dma("sp", lambda q: q.dma_start(out=wd[:, k, :], in_=wdn16_d[ex, k * 128:(k + 1) * 128, :]),
                                                  r=[WCV[ex][1]], w=[wdh[ex % 2][k]]))
                return pcs

            ids0 = [sb(st, f"ids0{i}", [128, 2], I32) for i in range(2)]
            xg0 = [sb(st, f"xg0{i}", [128, D], BF16) for i in range(2)]

            def blk_bufs(ex, j):
                nbidx = ex * nbmax + j
                return (ids0[ex % 2], xg0[ex % 2]) if j == 0 else (ids[nbidx % 3], xg[nbidx % 3])

            def issue_fetch(ex, j):
                idt, xgt = blk_bufs(ex, j)
                blk = ex * NBMAX + j
                fw.dma("pool", lambda q: q.dma_start(out=idt[:], in_=lists_d[blk * 128:(blk + 1) * 128, :]), r=[LISTS], w=[idt])
                fw.dma("pool", lambda q: q.indirect_dma_start(out=xgt[:], out_offset=None, in_=x2_d,
                                                              in_offset=bass.IndirectOffsetOnAxis(ap=idt[:, 0:1], axis=0)), r=[X2D, idt], w=[xgt])

            def emit_tx(ex, j):
                nbidx = ex * nbmax + j
                xgt, xT = blk_bufs(ex, j)[1], xgT[nbidx % 2]
                for k in range(8):
                    OP("pe", lambda e: e.transpose(out=pbb(0)[:, k * 128:(k + 1) * 128], in_=xgt[:, k * 128:(k + 1) * 128], identity=identb),
                       r=[xgt, cb], w=[PT0])
                OP("act", lambda e: e.activation(out=xT[:].rearrange("p k t -> p (k t)"), in_=pbb(0), func=AF.Copy), r=[PT0], w=[xT])

            while conv_todo:
                conv_step()
            load_bias(0)
            issue_fetch(0, 0)
            for pc in weight_pieces(0):
                pc()
            for ex in range(E):
                pcs = []
                if ex + 1 < E:
                    issue_fetch(ex + 1, 0)
                    load_bias(ex + 1)
                    pcs = weight_pieces(ex + 1)
                wg, wd = wgu[ex % 2], wdn[ex % 2]
                bhi = bhl[ex % 2][0]
                for e_ in fw.ENG:
                    fw.sync_to(e_, [nbi])
                for reg in regs:
                    nc.reg_load(reg, nbi[0:1, ex:ex + 1])

                def emit_block(j):
                    fw.begin_if_cmp(regs, j, "IS_GT")
                    nbidx = ex * nbmax + j
                    (idt, xgt), ywt = blk_bufs(ex, j), yw[nbidx % 2]
                    par = 0
                    xT = xgT[nbidx % 2]
                    if j + 1 < nbmax:
                        issue_fetch(ex, j + 1)
                    emit_tx(ex, j)
                    for hf in range(2):
                        for gi, c0 in enumerate((hf * 512, D + hf * 512)):
                            bk = 2 + 2 * hf + gi
                            for k in range(8):
                                OP("pe", lambda e: e.matmul(pb(bk), lhsT=xT[:, k, :], rhs=wg[:, k, c0:c0 + 512], start=(k == 0), stop=False),
                                   r=[xT, wgh[ex % 2][k]], w=[PGh[hf]])
                            OP("pe", lambda e: e.matmul(pb(bk), lhsT=ones1b, rhs=bhi[0:1, c0:c0 + 512], start=False, stop=True), r=[cb, bhi], w=[PGh[hf]])
                    for hf in range(2):
                        g_, s_, u_, a_, aT_ = gc[par][hf], sg[par][hf], uc[par][hf], actb[par][hf], actT[par][hf]
                        OP("dve", lambda e: e.tensor_scalar(out=g_[:], in0=pb(2 + 2 * hf), scalar1=7.0, scalar2=None, op0=ALU.min), r=[PGh[hf]], w=[g_])
                        OP("act", lambda e: e.activation(out=s_[:], in_=g_[:], func=AF.Sigmoid, scale=1.702), r=[g_], w=[s_])
                        OP("dve", lambda e: e.tensor_scalar(out=u_[:], in0=pb(3 + 2 * hf), scalar1=-7.0, scalar2=7.0, op0=ALU.max, op1=ALU.min), r=[PGh[hf]], w=[u_])
                        OP("dve", lambda e: e.scalar_tensor_tensor(out=u_[:], in0=u_[:], scalar=1.0, in1=g_[:], op0=ALU.add, op1=ALU.mult), r=[u_, g_], w=[u_])
                        OP("dve", lambda e: e.tensor_tensor(out=a_[:], in0=u_[:], in1=s_[:], op=ALU.mult), r=[u_, s_], w=[a_])
                        for c in range(4):
                            OP("pe", lambda e: e.transpose(out=pbb(1)[:, (hf * 4 + c) * 128:(hf * 4 + c + 1) * 128], in_=a_[:, c * 128:(c + 1) * 128], identity=identb),
                               r=[a_, cb], w=[PT1h[hf]])
                        OP("act", lambda e: e.activation(out=aT_[:].rearrange("p k t -> p (k t)"), in_=pbb(1)[:, hf * 512:(hf + 1) * 512], func=AF.Copy),
                           r=[PT1h[hf]], w=[aT_])
                        for n in range(2):
                            for k in range(4 * hf, 4 * hf + 4):
                                OP("pe", lambda e: e.matmul(pb(6 + n), lhsT=aT_[:, k % 4, :], rhs=wd[:, k, n * 512:(n + 1) * 512], start=(k == 0), stop=False),
                                   r=[aT_, wdh[ex % 2][k]], w=[PD])
                    for n in range(2):
                        OP("pe", lambda e: e.matmul(pb(6 + n), lhsT=ones1b, rhs=bhi[0:1, 2 * D + n * 512:2 * D + (n + 1) * 512], start=False, stop=True), r=[cb, bhi], w=[PD])
                    OP("act", lambda e: e.activation(out=ywt[:], in_=psum[:, 6 * 512:8 * 512], func=AF.Copy, scale=idt[:, 1:2].bitcast(F32)), r=[PD, idt], w=[ywt])
                    fw.dma("pool", lambda q: q.indirect_dma_start(out=yacc_d, out_offset=bass.IndirectOffsetOnAxis(ap=idt[:, 0:1], axis=0),
                                                                  in_=ywt[:], in_offset=None, compute_op=ALU.add), r=[ywt, idt, YACC], w=[YACC])

                while pcs:
                    pcs.pop(0)()
                for j in range(nbmax):
                    emit_block(j)
                for j in range(nbmax):
                    fw.end_if()
            fw.barrier()

        if phases >= 4:
          with ExitStack() as st:
            g2b = [sb(st, f"g2b{i}", [128, D], F32) for i in range(2)]
            fnwb = sb(st, "fnwb", [128, D], F32)
            NF = 4
            F_ = [dict(h=sb(st, f"fh{i}", [128, D], F32), y=sb(st, f"fy{i}", [128, D], F32), o=sb(st, f"fo{i}", [128, D], F32),
                       junk=sb(st, f"fj{i}", [128, D], BF16), tv=sb(st, f"ftv{i}", [128, 4], F32), out=T(None)) for i in range(NF)]
            fw.dma("sp", lambda q: q.dma_start(out=fnwb[:], in_=fnw_d), w=[fnwb])
            for b in range(2):
                fw.dma("sp", lambda q: q.dma_start(out=g2b[b][:], in_=mod_d[b:b + 1, 5 * D:6 * D].partition_broadcast(128)), r=[MODD], w=[g2b[b]])

            def final_gen(tg, slot):
                f_ = F_[slot]
                h_, y_, o_, jk, tv, OUT = f_["h"], f_["y"], f_["o"], f_["junk"], f_["tv"], f_["out"]
                rows = slice(tg * 128, (tg + 1) * 128)
                fw.dma("sp", lambda q: q.dma_start(out=h_[:], in_=h1_d[rows, :]), r=[H1D], w=[h_])
                fw.dma("sp", lambda q: q.dma_start(out=y_[:], in_=yacc_d[rows, :]), r=[YACC], w=[y_])
                yield
                OP("dve", lambda e: e.tensor_tensor(out=y_[:], in0=y_[:], in1=g2b[tg // 16][:], op=ALU.mult), r=[y_, g2b[tg // 16]], w=[y_])
                OP("dve", lambda e: e.tensor_tensor(out=h_[:], in0=h_[:], in1=y_[:], op=ALU.add), r=[h_, y_], w=[h_])
                OP("act", lambda e: e.activation(out=jk[:], in_=h_[:], func=AF.Square, accum_out=tv[:, 0:1]), r=[h_], w=[jk, tv])
                yield
                rstd_ss(tv, tv[:, 0:1], tv[:, 1:2], D)
                yield
                OP("dve", lambda e: e.scalar_tensor_tensor(out=o_[:], in0=h_[:], scalar=tv[:, 1:2], in1=fnwb[:], op0=ALU.mult, op1=ALU.mult),
                   r=[h_, tv, fnwb], w=[o_])
                fw.dma("sp", lambda q: q.dma_start(out=out_d[rows, :], in_=o_[:]), r=[o_], w=[OUT])

            run_pipelined(final_gen, list(range(NTILE)), nslots=NF, gap=1)
            fw.barrier()

    except _Stop:
        fw.barrier()
        return nc, fw
    root.close()
    return nc, fw


_WIN_PERM = np.concatenate([np.arange(0, 1536), np.arange(1568, 2080), np.arange(1536, 1568)])


def make_in_maps(x, c, ctx, c_ctx, w_ada, b_ada, norm_mix_w, norm_mlp_w, w_in, w_gk_f, b_gk_f, w_gk_b, b_gk_b,
                 gla_norm_w, w_pool, pool_scale, w_out, w_router, b_router, w_gu, b_gu, w_down, b_down, final_norm_w):
    f = lambda a: np.ascontiguousarray(np.asarray(a, dtype=np.float32))
    cf, cbf = _consts()
    wgk = np.zeros((32, 4, 2, 64), np.float32)
    wgk[0:16, :, 0, :] = f(w_gk_f)[0].reshape(16, 4, 64)
    wgk[16:32, :, 1, :] = f(w_gk_b)[0].reshape(16, 4, 64)
    bgk = np.stack([f(b_gk_f)[0].reshape(4, 64), f(b_gk_b)[0].reshape(4, 64)], axis=1).reshape(1, 512)
    rep = lambda v: np.ascontiguousarray(np.broadcast_to(f(v).reshape(1, -1), (128, f(v).size)))
    shared = {
        "w_ada": f(w_ada)[0], "b_ada": f(b_ada)[0].reshape(1, -1),
        "nmwT": np.ascontiguousarray(f(norm_mix_w)[0].reshape(8, 128).T), "nmlp_bc": rep(norm_mlp_w), "fnw_bc": rep(final_norm_w),
        "w_in": np.ascontiguousarray(f(w_in)[0][:, _WIN_PERM]), "wgk": wgk.reshape(32, 512), "bgk": np.ascontiguousarray(bgk),
        "gnw_bc": rep(np.tile(f(gla_norm_w)[0], 4)), "w_pool": f(w_pool)[0], "psc_bc": rep(pool_scale),
        "w_out": f(w_out)[0], "w_router": f(w_router)[0], "br_bc": rep(b_router),
        "w_gu": f(w_gu)[0], "b_gu": f(b_gu)[0], "w_down": f(w_down)[0], "b_down": f(b_down)[0],
        "cf": cf, "cb": cbf, "linit": _list_init(),
    }
    x = f(x); ctx = f(ctx); c = f(c); c_ctx = f(c_ctx)
    maps = []
    for i in range(NCORES):
        cv = np.stack([c[2 * i], c[2 * i + 1], c_ctx], axis=0)
        cT = np.ascontiguousarray(cv.reshape(3, 8, 128).transpose(2, 1, 0))
        m = dict(shared)
        m["x"] = x[2 * i:2 * i + 2].reshape(NT, D)
        m["ctx"] = ctx[2 * i:2 * i + 2].reshape(NCTX, D)
        m["cT"] = cT
        maps.append(m)
    return maps


def kernel(**inputs):
    nc, _ = build_program()
    maps = make_in_maps(**inputs)
    res = run_bass_kernel_spmd(nc, maps, core_ids=list(range(NCORES)))
    out = np.stack([np.asarray(r["out"]).reshape(2, 2048, D) for r in res.results], axis=0)
    return out.reshape(16, 2048, D).astype(np.float32)
```
